# Optimizing a Trainium2 kernel written in Bass

```python
import math
import jax
import jax.numpy as jnp
from jax import lax
import numpy as np

D_MODEL = 1024
BATCH = 4
SEQ = 8192
DEPTH = 2

GDN_HEADS = 4
GDN_DK = 128
GDN_DV = 128
GDN_CHUNK = 64
CONV_WIDTH = 4
MLSTM_HEADS = 4
MLSTM_DQK = 128
MLSTM_DV = 128
MLSTM_CHUNK = 64
GATE_CAP = 15.0
MLA_HEADS = 4
MLA_NOPE = 128
MLA_ROPE = 64
MLA_V = 128
MLA_Q_LORA = 384
MLA_KV_LORA = 256
ROPE_THETA = 10000.0
Q_BLOCK = 128
N_EXPERTS = 16
N_GROUPS = 4
EXPERTS_PER_GROUP = N_EXPERTS // N_GROUPS
TOP_K = 2
D_EXPERT = 512
EXPERT_BLOCK = 256
LN_EPS = 1e-5
RMS_EPS = 1e-6
DEEPNORM_ALPHA = (2 * DEPTH) ** 0.25
DEEPNORM_BETA = (8 * DEPTH) ** -0.25

GDN_QK_WIDTH = GDN_HEADS * GDN_DK
GDN_V_WIDTH = GDN_HEADS * GDN_DV
MLSTM_QK_WIDTH = MLSTM_HEADS * MLSTM_DQK
MLSTM_V_WIDTH = MLSTM_HEADS * MLSTM_DV
MLA_V_WIDTH = MLA_HEADS * MLA_V
N_BRANCHES = 3
IN_SIZES = (GDN_QK_WIDTH, GDN_QK_WIDTH, GDN_V_WIDTH, GDN_V_WIDTH, GDN_HEADS, GDN_HEADS,
            MLSTM_QK_WIDTH, MLSTM_QK_WIDTH, MLSTM_V_WIDTH, MLSTM_V_WIDTH, MLSTM_HEADS, MLSTM_HEADS,
            MLA_Q_LORA, MLA_KV_LORA, MLA_ROPE, N_BRANCHES * D_MODEL)
D_IN = (2 * GDN_QK_WIDTH + 2 * GDN_V_WIDTH + 2 * GDN_HEADS
        + 2 * MLSTM_QK_WIDTH + 2 * MLSTM_V_WIDTH + 2 * MLSTM_HEADS
        + MLA_Q_LORA + MLA_KV_LORA + MLA_ROPE + N_BRANCHES * D_MODEL)

kernel_name = 'hybrid_gdn_mlstm_mla_grouped_moe_deepnorm'


def layer_norm(x, g, b):
    xf = x.astype(jnp.float32)
    mu = jnp.mean(xf, axis=-1, keepdims=True)
    var = jnp.mean(jnp.square(xf - mu), axis=-1, keepdims=True)
    return ((xf - mu) * lax.rsqrt(var + LN_EPS) * g.astype(jnp.float32) + b.astype(jnp.float32)).astype(x.dtype)


def rms_norm(x, g):
    xf = x.astype(jnp.float32)
    y = xf * lax.rsqrt(jnp.mean(xf * xf, axis=-1, keepdims=True) + RMS_EPS)
    return (y * g.astype(jnp.float32)).astype(x.dtype)


def l2_normalize(x):
    return x * lax.rsqrt(jnp.sum(x * x, axis=-1, keepdims=True) + RMS_EPS)


def soft_cap(x):
    return GATE_CAP * jnp.tanh(x / GATE_CAP)


def split_columns(h):
    points = []
    acc = 0
    for size in IN_SIZES[:-1]:
        acc += size
        points.append(acc)
    return jnp.split(h, points, axis=-1)


def causal_depthwise_conv(x, w):
    width, ch = w.shape
    return lax.conv_general_dilated(
        x, w[:, None, :].astype(x.dtype), window_strides=(1,), padding=[(width - 1, 0)],
        dimension_numbers=('NWC', 'WIO', 'NWC'), feature_group_count=ch)


def to_chunks(x, size):
    b, s, h, d = x.shape
    return x.reshape(b, s // size, size, h, d).transpose(1, 0, 3, 2, 4)


def scalar_chunks(x, size):
    b, s, h = x.shape
    return x.reshape(b, s // size, size, h).transpose(1, 0, 3, 2)


def from_chunks(x):
    n, b, h, size, d = x.shape
    return x.transpose(1, 0, 3, 2, 4).reshape(b, n * size, h, d)


def rope_angles(positions, dim):
    inv_freq = 1.0 / (ROPE_THETA ** (jnp.arange(0, dim, 2, dtype=jnp.float32) / dim))
    ang = positions.astype(jnp.float32)[..., None] * inv_freq
    return jnp.cos(ang), jnp.sin(ang)


def apply_rope(x, cos, sin):
    x1, x2 = jnp.split(x, 2, axis=-1)
    return jnp.concatenate([x1 * cos - x2 * sin, x2 * cos + x1 * sin], axis=-1).astype(x.dtype)


def gated_deltanet(q, k, v, a, b, z, conv_w, a_log, dt_bias, norm_g):
    bsz, seq, _ = q.shape
    size = GDN_CHUNK
    qkv = jax.nn.silu(causal_depthwise_conv(jnp.concatenate([q, k, v], axis=-1), conv_w))
    q, k, v = jnp.split(qkv.astype(jnp.float32), [GDN_QK_WIDTH, 2 * GDN_QK_WIDTH], axis=-1)
    q = l2_normalize(q.reshape(bsz, seq, GDN_HEADS, GDN_DK)) * GDN_DK ** -0.5
    k = l2_normalize(k.reshape(bsz, seq, GDN_HEADS, GDN_DK))
    v = v.reshape(bsz, seq, GDN_HEADS, GDN_DV)
    g = -jnp.exp(a_log.astype(jnp.float32)) * jax.nn.softplus(a.astype(jnp.float32) + dt_bias.astype(jnp.float32))
    beta = jax.nn.sigmoid(b.astype(jnp.float32))

    qc, kc, vc = to_chunks(q, size), to_chunks(k, size), to_chunks(v, size)
    gcum = jnp.cumsum(scalar_chunks(g, size), axis=-1)
    betac = scalar_chunks(beta, size)
    idx = jnp.arange(size)
    causal = idx[:, None] >= idx[None, :]
    strict = idx[:, None] > idx[None, :]
    decay = jnp.exp(jnp.where(causal, gcum[..., :, None] - gcum[..., None, :], -jnp.inf))
    eye = jnp.eye(size, dtype=jnp.float32)
    a_mat = jnp.where(strict, jnp.einsum('nbhid,nbhjd->nbhij', kc * betac[..., None], kc) * decay, 0.0)
    t_mat = lax.linalg.triangular_solve(a_mat + eye, jnp.broadcast_to(eye, a_mat.shape),
                                        left_side=True, lower=True, unit_diagonal=True)
    u = jnp.einsum('nbhij,nbhjd->nbhid', t_mat, vc * betac[..., None])
    w = jnp.einsum('nbhij,nbhjd->nbhid', t_mat, kc * (betac * jnp.exp(gcum))[..., None])
    qk = jnp.einsum('nbhid,nbhjd->nbhij', qc, kc) * decay
    q_dec = qc * jnp.exp(gcum)[..., None]
    k_tail = kc * jnp.exp(gcum[..., -1:] - gcum)[..., None]
    g_last = jnp.exp(gcum[..., -1])

    def step(state, inp):
        u_c, w_c, qd_c, qk_c, kt_c, gl_c = inp
        v_new = u_c - jnp.einsum('bhld,bhde->bhle', w_c, state)
        out = jnp.einsum('bhld,bhde->bhle', qd_c, state) + jnp.einsum('bhij,bhje->bhie', qk_c, v_new)
        state = state * gl_c[..., None, None] + jnp.einsum('bhld,bhle->bhde', kt_c, v_new)
        return state, out

    s0 = jnp.zeros((bsz, GDN_HEADS, GDN_DK, GDN_DV), jnp.float32)
    _, o = lax.scan(step, s0, (u, w, q_dec, qk, k_tail, g_last))
    o = from_chunks(o)
    o = rms_norm(o, norm_g) * jax.nn.silu(z.astype(jnp.float32).reshape(bsz, seq, GDN_HEADS, GDN_DV))
    return o.reshape(bsz, seq, GDN_V_WIDTH).astype(z.dtype)


def mlstm(q, k, v, o_pre, i_pre, f_pre, gate_bias, norm_g):
    bsz, seq, _ = q.shape
    size = MLSTM_CHUNK
    q = q.astype(jnp.float32).reshape(bsz, seq, MLSTM_HEADS, MLSTM_DQK)
    k = k.astype(jnp.float32).reshape(bsz, seq, MLSTM_HEADS, MLSTM_DQK) * MLSTM_DQK ** -0.5
    v = v.astype(jnp.float32).reshape(bsz, seq, MLSTM_HEADS, MLSTM_DV)
    gb = gate_bias.astype(jnp.float32)
    i_log = soft_cap(i_pre.astype(jnp.float32) + gb[:MLSTM_HEADS])
    f_log = jax.nn.log_sigmoid(soft_cap(f_pre.astype(jnp.float32) + gb[MLSTM_HEADS:]))

    qc, kc, vc = to_chunks(q, size), to_chunks(k, size), to_chunks(v, size)
    ic = scalar_chunks(i_log, size)
    bcum = jnp.cumsum(scalar_chunks(f_log, size), axis=-1)
    idx = jnp.arange(size)
    causal = idx[:, None] >= idx[None, :]
    log_d = jnp.where(causal, bcum[..., :, None] - bcum[..., None, :] + ic[..., None, :], -jnp.inf)
    qk = jnp.einsum('nbhid,nbhjd->nbhij', qc, kc)
    log_kw = bcum[..., -1:] - bcum + ic
    b_last = bcum[..., -1]

    def step(carry, inp):
        c_st, n_st, m_st = carry
        q_c, k_c, v_c, ld_c, qk_c, b_c, lkw_c, bl_c = inp
        log_inter = b_c + m_st[..., None]
        m_t = jnp.maximum(log_inter, jnp.max(ld_c, axis=-1))
        w_inter = jnp.exp(log_inter - m_t)
        s = qk_c * jnp.exp(ld_c - m_t[..., None])
        num = w_inter[..., None] * jnp.einsum('bhld,bhde->bhle', q_c, c_st) + jnp.einsum('bhij,bhje->bhie', s, v_c)
        den = w_inter * jnp.einsum('bhld,bhd->bhl', q_c, n_st) + jnp.sum(s, axis=-1)
        h = num / jnp.maximum(jnp.abs(den), jnp.exp(-m_t))[..., None]
        m_new = jnp.maximum(bl_c + m_st, jnp.max(lkw_c, axis=-1))
        carry_decay = jnp.exp(bl_c + m_st - m_new)
        kw = jnp.exp(lkw_c - m_new[..., None])
        c_st = carry_decay[..., None, None] * c_st + jnp.einsum('bhld,bhle->bhde', k_c * kw[..., None], v_c)
        n_st = carry_decay[..., None] * n_st + jnp.einsum('bhl,bhld->bhd', kw, k_c)
        return (c_st, n_st, m_new), h

    init = (jnp.zeros((bsz, MLSTM_HEADS, MLSTM_DQK, MLSTM_DV), jnp.float32),
            jnp.zeros((bsz, MLSTM_HEADS, MLSTM_DQK), jnp.float32),
            jnp.zeros((bsz, MLSTM_HEADS), jnp.float32))
    _, h = lax.scan(step, init, (qc, kc, vc, log_d, qk, bcum, log_kw, b_last))
    h = rms_norm(from_chunks(h), norm_g.reshape(MLSTM_HEADS, MLSTM_DV))
    h = jax.nn.sigmoid(o_pre.astype(jnp.float32)).reshape(bsz, seq, MLSTM_HEADS, MLSTM_DV) * h
    return h.reshape(bsz, seq, MLSTM_V_WIDTH).astype(o_pre.dtype)


def latent_attention(c_q, c_kv, k_rope, positions, q_norm_g, kv_norm_g, w_uq, w_ukv):
    bsz, seq, _ = c_q.shape
    q = (rms_norm(c_q, q_norm_g) @ w_uq).reshape(bsz, seq, MLA_HEADS, MLA_NOPE + MLA_ROPE)
    kv = (rms_norm(c_kv, kv_norm_g) @ w_ukv).reshape(bsz, seq, MLA_HEADS, MLA_NOPE + MLA_V)
    q_nope, q_rope = jnp.split(q, [MLA_NOPE], axis=-1)
    k_nope, v = jnp.split(kv, [MLA_NOPE], axis=-1)
    cos, sin = rope_angles(positions, MLA_ROPE)
    q_rope = apply_rope(q_rope, cos[:, :, None, :], sin[:, :, None, :])
    k_rope = apply_rope(k_rope, cos, sin)
    scale = (MLA_NOPE + MLA_ROPE) ** -0.5
    n_blk = seq // Q_BLOCK

    def blocks(t):
        return t.reshape(bsz, n_blk, Q_BLOCK, MLA_HEADS, t.shape[-1]).transpose(1, 0, 2, 3, 4)

    key_idx = jnp.arange(seq)

    def attend(args):
        qn, qr, blk = args
        s = jnp.einsum('bqhd,bkhd->bhqk', qn, k_nope) + jnp.einsum('bqhd,bkd->bhqk', qr, k_rope)
        q_idx = blk * Q_BLOCK + jnp.arange(Q_BLOCK)
        s = jnp.where(q_idx[:, None] >= key_idx[None, :], s.astype(jnp.float32) * scale, -jnp.inf)
        p = jax.nn.softmax(s, axis=-1).astype(v.dtype)
        return jnp.einsum('bhqk,bkhd->bqhd', p, v)

    out = lax.map(attend, (blocks(q_nope), blocks(q_rope), jnp.arange(n_blk)))
    return out.transpose(1, 0, 2, 3, 4).reshape(bsz, seq, MLA_V_WIDTH)


def token_mixer(x, positions, w_in, gdn_conv, gdn_a_log, gdn_dt_bias, gdn_norm, mlstm_gate_bias, mlstm_norm,
                mla_q_norm, mla_kv_norm, mla_w_uq, mla_w_ukv, w_br_gdn, w_br_mlstm, w_br_mla, gate_bias, w_out):
    bsz, seq, _ = x.shape
    (g_q, g_k, g_v, g_z, g_a, g_b, m_q, m_k, m_v, m_o, m_i, m_f,
     c_q, c_kv, k_rope, gate_logits) = split_columns(x @ w_in)
    y_gdn = gated_deltanet(g_q, g_k, g_v, g_a, g_b, g_z, gdn_conv, gdn_a_log, gdn_dt_bias, gdn_norm)
    y_mlstm = mlstm(m_q, m_k, m_v, m_o, m_i, m_f, mlstm_gate_bias, mlstm_norm)
    y_mla = latent_attention(c_q, c_kv, k_rope, positions, mla_q_norm, mla_kv_norm, mla_w_uq, mla_w_ukv)
    gates = jax.nn.sigmoid((gate_logits + gate_bias).astype(jnp.float32)).astype(x.dtype)
    gates = gates.reshape(bsz, seq, N_BRANCHES, D_MODEL)
    merged = (gates[:, :, 0] * (y_gdn @ w_br_gdn)
              + gates[:, :, 1] * (y_mlstm @ w_br_mlstm)
              + gates[:, :, 2] * (y_mla @ w_br_mla))
    return merged @ w_out


def route(x_flat, router_w, router_bias):
    n_tok = x_flat.shape[0]
    scores = jax.nn.sigmoid((x_flat @ router_w).astype(jnp.float32))
    biased = (scores + router_bias.astype(jnp.float32)).reshape(n_tok, N_GROUPS, EXPERTS_PER_GROUP)
    group_score = jnp.sum(lax.top_k(biased, TOP_K)[0], axis=-1)
    group = jnp.argmax(group_score, axis=-1).astype(jnp.int32)
    in_group = biased[jnp.arange(n_tok), group]
    _, local = lax.top_k(in_group, TOP_K)
    expert = group[:, None] * EXPERTS_PER_GROUP + local.astype(jnp.int32)
    weight = jnp.take_along_axis(scores, expert, axis=1)
    weight = weight / jnp.sum(weight, axis=-1, keepdims=True)
    return expert, weight


def routed_experts(x, router_w, router_bias, w1, w3, w2):
    bsz, seq, d = x.shape
    n_tok = bsz * seq
    n_assign = n_tok * TOP_K
    x_flat = x.reshape(n_tok, d)
    expert, weight = route(x_flat, router_w, router_bias)
    e_flat = expert.reshape(n_assign)
    tok_flat = jnp.repeat(jnp.arange(n_tok, dtype=jnp.int32), TOP_K)
    order = jnp.argsort(e_flat)
    e_sorted = e_flat[order]
    tok_sorted = tok_flat[order]
    w_sorted = weight.reshape(n_assign)[order]
    counts = jnp.bincount(e_flat, length=N_EXPERTS)
    starts = jnp.cumsum(counts) - counts
    padded = (counts + EXPERT_BLOCK - 1) // EXPERT_BLOCK * EXPERT_BLOCK
    padded_ends = jnp.cumsum(padded)
    padded_starts = padded_ends - padded
    dest = padded_starts[e_sorted] + jnp.arange(n_assign, dtype=jnp.int32) - starts[e_sorted]
    n_rows = n_assign + N_EXPERTS * EXPERT_BLOCK
    n_blocks = n_rows // EXPERT_BLOCK
    row_tok = jnp.zeros((n_rows,), jnp.int32).at[dest].set(tok_sorted)
    row_w = jnp.zeros((n_rows,), jnp.float32).at[dest].set(w_sorted)
    block_start = jnp.arange(n_blocks, dtype=jnp.int32) * EXPERT_BLOCK
    block_expert = jnp.minimum(jnp.searchsorted(padded_ends, block_start, side='right'), N_EXPERTS - 1)
    x_rows = x_flat[row_tok].reshape(n_blocks, EXPERT_BLOCK, d)

    def expert_ffn(args):
        xb, e = args
        return (jax.nn.silu(xb @ w1[e]) * (xb @ w3[e])) @ w2[e]

    y_rows = lax.map(expert_ffn, (x_rows, block_expert)).reshape(n_rows, d)
    out = jnp.zeros((n_tok, d), x.dtype).at[row_tok].add((y_rows * row_w[:, None]).astype(x.dtype))
    return out.reshape(bsz, seq, d)


def setup_inputs(seed: int = 0) -> dict:
    key = jax.random.key(seed)
    ks = jax.random.split(key, 32)
    f32 = jnp.float32

    def nrm(k, shape, scale):
        return jax.random.normal(k, shape, f32) * scale

    x = nrm(ks[0], (BATCH, SEQ, D_MODEL), 1.0)
    offsets = jax.random.randint(ks[1], (BATCH, 1), 0, SEQ, dtype=jnp.int32)
    positions = offsets + jnp.arange(SEQ, dtype=jnp.int32)[None, :]
    ln_in_g = 1.0 + nrm(ks[2], (D_MODEL,), 0.02)
    ln_in_b = nrm(ks[3], (D_MODEL,), 0.02)
    w_in = nrm(ks[4], (DEPTH, D_MODEL, D_IN), D_MODEL ** -0.5)
    gdn_conv = nrm(ks[5], (DEPTH, CONV_WIDTH, 2 * GDN_QK_WIDTH + GDN_V_WIDTH), CONV_WIDTH ** -0.5)
    gdn_a_log = jnp.log(jax.random.uniform(ks[6], (DEPTH, GDN_HEADS), f32, 1.0, 16.0))
    dt = jnp.exp(jax.random.uniform(ks[7], (DEPTH, GDN_HEADS), f32, math.log(1e-3), math.log(1e-1)))
    gdn_dt_bias = dt + jnp.log(-jnp.expm1(-dt))
    gdn_norm = 1.0 + nrm(ks[8], (DEPTH, GDN_DV), 0.02)
    mlstm_gate_bias = jnp.concatenate([nrm(ks[9], (DEPTH, MLSTM_HEADS), 0.1),
                                       3.0 + nrm(ks[10], (DEPTH, MLSTM_HEADS), 0.5)], axis=-1)
    mlstm_norm = 1.0 + nrm(ks[11], (DEPTH, MLSTM_V_WIDTH), 0.02)
    mla_q_norm = 1.0 + nrm(ks[12], (DEPTH, MLA_Q_LORA), 0.02)
    mla_kv_norm = 1.0 + nrm(ks[13], (DEPTH, MLA_KV_LORA), 0.02)
    mla_w_uq = nrm(ks[14], (DEPTH, MLA_Q_LORA, MLA_HEADS * (MLA_NOPE + MLA_ROPE)), MLA_Q_LORA ** -0.5)
    mla_w_ukv = nrm(ks[15], (DEPTH, MLA_KV_LORA, MLA_HEADS * (MLA_NOPE + MLA_V)), MLA_KV_LORA ** -0.5)
    w_br_gdn = nrm(ks[16], (DEPTH, GDN_V_WIDTH, D_MODEL), GDN_V_WIDTH ** -0.5)
    w_br_mlstm = nrm(ks[17], (DEPTH, MLSTM_V_WIDTH, D_MODEL), MLSTM_V_WIDTH ** -0.5)
    w_br_mla = nrm(ks[18], (DEPTH, MLA_V_WIDTH, D_MODEL), MLA_V_WIDTH ** -0.5)
    gate_bias = nrm(ks[19], (DEPTH, N_BRANCHES * D_MODEL), 0.02)
    w_out = nrm(ks[20], (DEPTH, D_MODEL, D_MODEL), DEEPNORM_BETA * D_MODEL ** -0.5)
    ln1_g = 1.0 + nrm(ks[21], (DEPTH, D_MODEL), 0.02)
    ln1_b = nrm(ks[22], (DEPTH, D_MODEL), 0.02)
    router_w = nrm(ks[23], (D_MODEL, N_EXPERTS), D_MODEL ** -0.5)
    router_bias = nrm(ks[24], (N_EXPERTS,), 0.01)
    moe_w1 = nrm(ks[25], (DEPTH, N_EXPERTS, D_MODEL, D_EXPERT), D_MODEL ** -0.5)
    moe_w3 = nrm(ks[26], (DEPTH, N_EXPERTS, D_MODEL, D_EXPERT), D_MODEL ** -0.5)
    moe_w2 = nrm(ks[27], (DEPTH, N_EXPERTS, D_EXPERT, D_MODEL), DEEPNORM_BETA * D_EXPERT ** -0.5)
    ln2_g = 1.0 + nrm(ks[28], (DEPTH, D_MODEL), 0.02)
    ln2_b = nrm(ks[29], (DEPTH, D_MODEL), 0.02)
    return {'x': x, 'positions': positions, 'ln_in_g': ln_in_g, 'ln_in_b': ln_in_b, 'w_in': w_in,
            'gdn_conv': gdn_conv, 'gdn_a_log': gdn_a_log, 'gdn_dt_bias': gdn_dt_bias, 'gdn_norm': gdn_norm,
            'mlstm_gate_bias': mlstm_gate_bias, 'mlstm_norm': mlstm_norm,
            'mla_q_norm': mla_q_norm, 'mla_kv_norm': mla_kv_norm, 'mla_w_uq': mla_w_uq, 'mla_w_ukv': mla_w_ukv,
            'w_br_gdn': w_br_gdn, 'w_br_mlstm': w_br_mlstm, 'w_br_mla': w_br_mla, 'gate_bias': gate_bias,
            'w_out': w_out, 'ln1_g': ln1_g, 'ln1_b': ln1_b, 'router_w': router_w, 'router_bias': router_bias,
            'moe_w1': moe_w1, 'moe_w3': moe_w3, 'moe_w2': moe_w2, 'ln2_g': ln2_g, 'ln2_b': ln2_b}


def reference(x, positions, ln_in_g, ln_in_b, w_in, gdn_conv, gdn_a_log, gdn_dt_bias, gdn_norm,
              mlstm_gate_bias, mlstm_norm, mla_q_norm, mla_kv_norm, mla_w_uq, mla_w_ukv,
              w_br_gdn, w_br_mlstm, w_br_mla, gate_bias, w_out, ln1_g, ln1_b, router_w, router_bias,
              moe_w1, moe_w3, moe_w2, ln2_g, ln2_b):
    x = layer_norm(x, ln_in_g, ln_in_b)
    for l in range(DEPTH):
        mix = token_mixer(x, positions, w_in[l], gdn_conv[l], gdn_a_log[l], gdn_dt_bias[l], gdn_norm[l],
                          mlstm_gate_bias[l], mlstm_norm[l], mla_q_norm[l], mla_kv_norm[l],
                          mla_w_uq[l], mla_w_ukv[l], w_br_gdn[l], w_br_mlstm[l], w_br_mla[l],
                          gate_bias[l], w_out[l])
        x = layer_norm(DEEPNORM_ALPHA * x + mix, ln1_g[l], ln1_b[l])
        ffn = routed_experts(x, router_w, router_bias, moe_w1[l], moe_w3[l], moe_w2[l])
        x = layer_norm(DEEPNORM_ALPHA * x + ffn, ln2_g[l], ln2_b[l])
    return x
```

```python
import contextlib
import math
import numpy as np
import concourse.bass as bass
import concourse.mybir as mybir
from concourse.bass_utils import run_bass_kernel_spmd

F32 = mybir.dt.float32
BF16 = mybir.dt.bfloat16
I32 = mybir.dt.int32
ALU = mybir.AluOpType
AF = mybir.ActivationFunctionType
AX = mybir.AxisListType

S = 8192
D = 1024
NT = S // 128
DEPTH = 2
DBG = {}
ALPHA = (2 * DEPTH) ** 0.25
NFEAT = 6400
NTOK = 1536
NW = NFEAT + NTOK + 16
R_GQ, R_GK, R_GV, R_MQ, R_MK, R_CQ, R_CKV, R_KR, R_KRS, R_GATE = 0, 512, 1024, 1536, 2048, 2560, 2944, 3200, 3264, 3328
C_GZ, C_MV, C_MO = 0, 512, 1024


class Buf:
    __slots__ = ("ap", "w", "r", "name")

    def __init__(self, ap, name=""):
        self.ap = ap
        self.w = None
        self.r = {}
        self.name = name

    def __getitem__(self, idx):
        return self.ap[idx]


class Eng:
    def __init__(self, name, eng, sem):
        self.name = name
        self.eng = eng
        self.sem = sem
        self.n = 0
        self.seen = {}


class K:
    def __init__(self, nc, n_dma_sems=8):
        self.nc = nc
        self.stack = contextlib.ExitStack()
        self.engs = {}
        for name, eng in (("pe", nc.tensor), ("act", nc.scalar), ("dve", nc.vector), ("pool", nc.gpsimd)):
            sem = self.stack.enter_context(nc.semaphore("s_" + name))
            self.engs[name] = Eng(name, eng, sem)
        self.engs["sync"] = Eng("sync", nc.sync, None)
        self.dmaq = {}
        for qname, eng, iss in (("sync", nc.sync, "sync"), ("gp", nc.gpsimd, "pool")):
            sems = [self.stack.enter_context(nc.semaphore(f"d_{qname}{i}")) for i in range(n_dma_sems)]
            self.dmaq[qname] = dict(eng=eng, sems=sems, vals=[0] * n_dma_sems, nxt=0, issuer=iss)
        self.dma_id = 0
        self.rr = 0
        self.epoch = 0

    def _wait(self, issuer, dep):
        if dep is None:
            return
        if dep[0] == "e":
            _, ename, seq, ep = dep
            if ep < self.epoch:
                return
            if ename == issuer.name and ename == "pe":
                return
            if issuer.seen.get(ename, 0) >= seq:
                return
            issuer.seen[ename] = seq
            issuer.eng.wait_ge(self.engs[ename].sem, seq)
        else:
            _, sem, val, tok = dep
            key = ("d", tok)
            if issuer.seen.get(key):
                return
            issuer.seen[key] = True
            issuer.eng.wait_ge(sem, val)

    def _deps(self, issuer, reads, writes):
        for b in reads:
            self._wait(issuer, b.w)
        for b in writes:
            self._wait(issuer, b.w)
            for d in list(b.r.values()):
                self._wait(issuer, d)

    def _mark(self, dep, reads, writes):
        key = dep[1] if dep[0] == "e" else ("d", dep[3])
        for b in reads:
            b.r[key] = dep
        for b in writes:
            b.w = dep
            b.r = {}

    def op(self, ename, fn, reads=(), writes=()):
        e = self.engs[ename]
        self._deps(e, reads, writes)
        ins = fn(e.eng)
        e.n += 1
        ins.then_inc(e.sem, 1)
        self._mark(("e", ename, e.n, self.epoch), reads, writes)
        return ins

    def ev(self, fn, reads=(), writes=()):
        return self.op("dve", fn, reads, writes)

    def dma(self, qname, out, in_, reads=(), writes=(), **kw):
        q = self.dmaq[qname]
        issuer = self.engs[q["issuer"]]
        i = q["nxt"]
        q["nxt"] = (i + 1) % len(q["sems"])
        sem = q["sems"][i]
        if q["vals"][i] > 0:
            issuer.eng.wait_ge(sem, q["vals"][i])
        self._deps(issuer, reads, writes)
        ins = q["eng"].dma_start(out=out, in_=in_, **kw)
        q["vals"][i] += 16
        ins.then_inc(sem, 16)
        self.dma_id += 1
        dep = ("d", sem, q["vals"][i], self.dma_id)
        self._mark(dep, reads, writes)
        return dep

    def barrier(self):
        for iss in self.engs.values():
            for e in self.engs.values():
                if e.sem is not None and e.n > 0 and e is not iss:
                    if iss.seen.get(e.name, 0) < e.n:
                        iss.seen[e.name] = e.n
                        iss.eng.wait_ge(e.sem, e.n)
            for q in self.dmaq.values():
                for sem, v in zip(q["sems"], q["vals"]):
                    if v > 0:
                        iss.eng.wait_ge(sem, v)

    def new_epoch(self):
        self.barrier()
        self.epoch += 1
        for name, e in self.engs.items():
            if e.sem is not None:
                e.sem = self.stack.enter_context(self.nc.semaphore(f"s_{name}_{self.epoch}"))
                e.n = 0
            e.seen = {k_: v for k_, v in e.seen.items() if isinstance(k_, tuple)}

    def finish(self):
        self.barrier()
        self.stack.close()


class Pool:
    cnt = 0

    def __init__(self, nc):
        self.nc = nc
        self.st = contextlib.ExitStack()
        self.i = 0

    def sb(self, shape, dt, name=None):
        Pool.cnt += 1
        name = f"{name or 't'}_{Pool.cnt}"
        return Buf(self.st.enter_context(self.nc.sbuf_tensor(name, list(shape), dt)), name)

    def ps(self, shape, dt, name=None):
        Pool.cnt += 1
        name = f"{name or 'p'}_{Pool.cnt}"
        full = 2048 // (2 if dt == BF16 else 4)
        t = self.st.enter_context(self.nc.psum_tensor(name, [shape[0], full], dt))
        return Buf(t[:, 0:shape[1]], name)

    def close(self):
        self.st.close()


class Rot:
    def __init__(self, items):
        self.items = items
        self.i = 0

    def next(self):
        b = self.items[self.i % len(self.items)]
        self.i += 1
        return b


def layer_norm_tile(k, P, xin, out, gbc, bbc, sc, eps=1e-5, pre=None):
    s1, nm, ssq, rstd, junk = sc["s1"], sc["nm"], sc["ssq"], sc["rstd"], sc["junk"]
    k.op("act", lambda e: e.activation(junk[:], xin[:], AF.Copy, accum_out=s1[:]), reads=[xin], writes=[junk, s1])
    k.op("dve", lambda e: e.tensor_scalar(nm[:], s1[:], -1.0 / D, None, ALU.mult), reads=[s1], writes=[nm])
    k.op("act", lambda e: e.activation(junk[:], xin[:], AF.Square, bias=nm[:], accum_out=ssq[:]), reads=[xin, nm], writes=[junk, ssq])
    k.op("dve", lambda e: e.tensor_scalar(rstd[:], ssq[:], 1.0 / D, eps, ALU.mult, ALU.add), reads=[ssq], writes=[rstd])
    k.op("act", lambda e: e.activation(rstd[:], rstd[:], AF.Sqrt), reads=[rstd], writes=[rstd])
    k.op("dve", lambda e: e.reciprocal(rstd[:], rstd[:]), reads=[rstd], writes=[rstd])
    k.op("dve", lambda e: e.tensor_scalar(out[:], xin[:], nm[:], rstd[:], ALU.add, ALU.mult), reads=[xin, nm, rstd], writes=[out])
    k.op("pool", lambda e: e.tensor_tensor(out[:], out[:], gbc[:], ALU.mult), reads=[out, gbc], writes=[out])
    k.op("pool", lambda e: e.tensor_tensor(out[:], out[:], bbc[:], ALU.add), reads=[out, bbc], writes=[out])


def ln_scratch(P):
    return dict(s1=P.sb([128, 1], F32), nm=P.sb([128, 1], F32), ssq=P.sb([128, 1], F32),
                rstd=P.sb([128, 1], F32), junk=P.sb([128, D], F32))


def phase1(k, nc, T, L):
    P = Pool(nc)
    first = (L == 0)
    W = [P.sb([128, NW], BF16, "W") for _ in range(8)]
    for kk in range(8):
        for h0 in range(0, NW, 1988):
            k.dma("gp", W[kk][:, h0:h0 + 1988], T["win"][L, kk * 128:(kk + 1) * 128, h0:h0 + 1988], writes=[W[kk]])
    ident = P.sb([128, 128], BF16, "ident")
    k.dma("gp", ident[:], T["c_ident"][:, :], writes=[ident])
    if first:
        gbc = P.sb([128, D], F32)
        bbc = P.sb([128, D], F32)
        k.dma("sync", gbc[:], T["ln_in_g"].partition_broadcast(128), writes=[gbc])
        k.dma("sync", bbc[:], T["ln_in_b"].partition_broadcast(128), writes=[bbc])
        sc = ln_scratch(P)
        xn_r = Rot([P.sb([128, D], F32, "xn") for _ in range(2)])
    xin_r = Rot([P.sb([128, D], F32, "xin") for _ in range(2)])
    xb_r = Rot([P.sb([128, D], BF16, "xb") for _ in range(2)])
    xT_r = Rot([P.sb([128, 8, 512], BF16, "xT") for _ in range(2)])
    tp_r = Rot([P.ps([128, D], BF16, "tp") for _ in range(2)])
    pp_r = Rot([P.ps([128, 512], F32, "pp") for _ in range(4)])
    stf_r = Rot([P.sb([128, 512], BF16, "stf") for _ in range(4)])
    stt_r = Rot([P.sb([128, NTOK], BF16, "stt") for _ in range(2)])
    sts_r = Rot([P.sb([128, 16], F32, "sts") for _ in range(2)])
    src = T["x"] if first else T["xres"]
    evi = 0
    for ti in range(S // 512):
        xT = xT_r.next()
        for s in range(4):
            r0 = ti * 512 + s * 128
            xi = xin_r.next()
            k.dma("sync", xi[:], src[r0:r0 + 128, :], writes=[xi])
            if first:
                xn = xn_r.next()
                layer_norm_tile(k, P, xi, xn, gbc, bbc, sc)
                k.dma("sync", T["xres"][r0:r0 + 128, :], xn[:], reads=[xn])
                xi = xn
            xb = xb_r.next()
            k.op("act", lambda e: e.activation(xb[:], xi[:], AF.Copy), reads=[xi], writes=[xb])
            tp = tp_r.next()
            for kk in range(8):
                k.op("pe", lambda e: e.transpose(tp[:, kk * 128:(kk + 1) * 128], xb[:, kk * 128:(kk + 1) * 128], ident[:]),
                     reads=[xb, ident], writes=[tp])
            k.op("dve", lambda e: e.tensor_copy(xT[:, :, s * 128:(s + 1) * 128], tp[:, :].rearrange("p (k t) -> p k t", k=8)),
                 reads=[tp], writes=[xT])
        for c in range(NFEAT // 128):
            pp = pp_r.next()
            for kk in range(8):
                k.op("pe", lambda e: e.matmul(pp[:], W[kk][:, c * 128:(c + 1) * 128], xT[:, kk, :], start=(kk == 0), stop=(kk == 7)),
                     reads=[W[kk], xT], writes=[pp])
            stf = stf_r.next()
            evi += 1
            if evi % 2:
                k.op("act", lambda e: e.activation(stf[:], pp[:], AF.Copy), reads=[pp], writes=[stf])
            else:
                k.op("dve", lambda e: e.tensor_copy(stf[:], pp[:]), reads=[pp], writes=[stf])
            k.dma("sync", T["F"][c * 128:(c + 1) * 128, ti * 512:(ti + 1) * 512], stf[:], reads=[stf])
        for s in range(4):
            r0 = ti * 512 + s * 128
            stt = stt_r.next()
            sts = sts_r.next()
            for j in range(4):
                pp = pp_r.next()
                n = 512 if j < 3 else 16
                c0 = NFEAT + j * 512
                for kk in range(8):
                    k.op("pe", lambda e: e.matmul(pp[:, :n], xT[:, kk, s * 128:(s + 1) * 128], W[kk][:, c0:c0 + n], start=(kk == 0), stop=(kk == 7)),
                         reads=[W[kk], xT], writes=[pp])
                evi += 1
                dst = stt[:, j * 512:(j + 1) * 512] if j < 3 else sts[:]
                dbuf = stt if j < 3 else sts
                if evi % 2:
                    k.op("act", lambda e: e.activation(dst, pp[:, :n], AF.Copy), reads=[pp], writes=[dbuf])
                else:
                    k.op("dve", lambda e: e.tensor_copy(dst, pp[:, :n]), reads=[pp], writes=[dbuf])
            k.dma("sync", T["Th"][r0:r0 + 128, :], stt[:], reads=[stt])
            k.dma("sync", T["Ts"][r0:r0 + 128, :], sts[:], reads=[sts])
    k.new_epoch()
    P.close()


def load_consts(k, P, T):
    CM = P.sb([128, 7, 128], F32, "CM")
    k.dma("sync", CM[:], T["c_mats"][:, :, :], writes=[CM])
    identb = P.sb([128, 128], BF16, "identb")
    k.dma("gp", identb[:], T["c_ident"][:, :], writes=[identb])
    onesb = P.sb([128, 128], BF16, "onesb")
    k.dma("gp", onesb[:], T["c_mats"][:, 4, :], writes=[onesb])
    return CM, identb, onesb


def rsqrt_inplace(k, buf, ap):
    k.op("act", lambda e: e.activation(ap, ap, AF.Sqrt), reads=[buf], writes=[buf])
    k.op("dve", lambda e: e.reciprocal(ap, ap), reads=[buf], writes=[buf])


def load_ts(k, P, T):
    TS = P.sb([128, NT, 16], F32, "TS")
    src = T["Ts"].rearrange("(c p) j -> p c j", p=128)
    for q in range(4):
        k.dma("sync", TS[:, q * 16:(q + 1) * 16, :], src[:, q * 16:(q + 1) * 16, :], writes=[TS])
    return TS


def phase2(k, nc, T, L):
    P = Pool(nc)
    CM, identb, onesb = load_consts(k, P, T)
    IDENT, UTI, LTS, UTS, ONES = (CM[:, i, :] for i in range(5))
    alog = P.sb([128, 4], F32)
    dtb = P.sb([128, 4], F32)
    normg = P.sb([128, 128], F32)
    convw = P.sb([128, 12, 4], F32)
    k.dma("sync", alog[:], T["gdn_a_log"][L].partition_broadcast(128), writes=[alog])
    k.dma("sync", dtb[:], T["gdn_dt_bias"][L].partition_broadcast(128), writes=[dtb])
    k.dma("sync", normg[:], T["gdn_norm"][L].partition_broadcast(128), writes=[normg])
    k.dma("sync", convw[:], T["gdn_convT"][L], writes=[convw])
    negA = P.sb([128, 4], F32)
    k.op("act", lambda e: e.activation(negA[:], alog[:], AF.Exp), reads=[alog], writes=[negA])
    k.op("dve", lambda e: e.tensor_scalar(negA[:], negA[:], -1.0, None, ALU.mult), reads=[negA], writes=[negA])
    TS = load_ts(k, P, T)
    G = P.sb([128, NT, 4], F32, "G")
    BETA = P.sb([128, NT, 4], F32, "BETA")
    NBETA = P.sb([128, NT, 4], F32, "NBETA")
    t1 = P.sb([128, NT, 4], F32)
    t2 = P.sb([128, NT, 4], F32)
    for h in range(4):
        k.op("dve", lambda e: e.tensor_scalar(G[:, :, h], TS[:, :, h], dtb[:, h:h + 1], None, ALU.add), reads=[TS, dtb], writes=[G])
    k.op("act", lambda e: e.activation(t1[:], G[:], AF.Abs), reads=[G], writes=[t1])
    k.op("act", lambda e: e.activation(t1[:], t1[:], AF.Exp, scale=-1.0), reads=[t1], writes=[t1])
    k.op("dve", lambda e: e.tensor_scalar(t1[:], t1[:], 1.0, None, ALU.add), reads=[t1], writes=[t1])
    k.op("act", lambda e: e.activation(t1[:], t1[:], AF.Ln), reads=[t1], writes=[t1])
    k.op("dve", lambda e: e.scalar_tensor_tensor(t2[:], G[:], 0.0, t1[:], ALU.max, ALU.add), reads=[G, t1], writes=[t2])
    for h in range(4):
        k.op("dve", lambda e: e.tensor_scalar(G[:, :, h], t2[:, :, h], negA[:, h:h + 1], None, ALU.mult), reads=[t2, negA], writes=[G])
    k.op("act", lambda e: e.activation(BETA[:], TS[:, :, 4:8], AF.Sigmoid), reads=[TS], writes=[BETA])
    k.op("dve", lambda e: e.tensor_scalar(NBETA[:], BETA[:], -1.0, None, ALU.mult), reads=[BETA], writes=[NBETA])

    if DBG.get("p2stop") == 0:
        k.barrier(); P.close(); return
    pre = [P.sb([128, 3 + S], BF16, "pre") for _ in range(3)]
    post = [P.sb([128, S], BF16, "post") for _ in range(3)]
    ZS = P.sb([128, NT, 128], BF16, "ZS")
    for i in range(3):
        k.op("pool", lambda e: e.memset(pre[i][:, 0:3], 0.0), writes=[pre[i]])
    acc_r = Rot([P.sb([128, 512], F32, "acc") for _ in range(2)])
    sl_r = Rot([P.sb([128, 512], F32, "sl") for _ in range(2)])
    sq_r = Rot([P.sb([128, 512], BF16, "sq") for _ in range(2)])
    rn_r = Rot([P.sb([128, 512], F32, "rn") for _ in range(2)])
    pBig = P.ps([128, 512], F32, "pBig")
    pA = P.ps([128, 256], F32, "pA")
    pT = P.ps([128, 256], BF16, "pT")
    pK = P.ps([128, 256], F32, "pK")
    pI_r = Rot([P.ps([128, 128], F32, "pI") for _ in range(2)])
    pS1 = P.ps([128, 128], F32, "pS1")
    pS2 = P.ps([128, 128], F32, "pS2")

    def f32t(n, name):
        return Rot([P.sb([128, 128], F32, name) for _ in range(n)])

    def bft(n, name):
        return Rot([P.sb([128, 128], BF16, name) for _ in range(n)])

    Gm_r, E_r = f32t(2, "Gm"), Rot([P.sb([128, 256], F32, "E") for _ in range(2)])
    DTc_r, DTs_r, Y_r = f32t(2, "DTc"), f32t(2, "DTs"), f32t(2, "Y")
    W_r, Z_r, IZ_r, Pm_r = f32t(3, "W"), f32t(3, "Z"), f32t(2, "IZ"), f32t(3, "Pm")
    u_r = f32t(2, "u")
    kg_r, kt_r, vt_r, P2_r, T2_r, wT_r, qd_r, vn_r, y_r, gz_r = (bft(2, n) for n in ("kg", "kt", "vt", "P2", "T2", "wT", "qd", "vn", "ych", "gz"))
    junk = P.sb([128, 128], F32, "junk")
    col_r = Rot([P.sb([128, 4], F32, "col") for _ in range(3)])
    Sst = P.sb([128, 128], F32, "Sst")
    Sbf = P.sb([128, 128], BF16, "Sbf")
    Fsc = T["F"]
    Thv = T["Th"].rearrange("(c p) d -> p c d", p=128)
    for h in range(DBG.get("nh", 4)):
        for i, r0 in enumerate((R_GQ, R_GK, R_GV)):
            for q in range(4):
                k.dma("sync", pre[i][:, 3 + q * 2048:3 + (q + 1) * 2048], Fsc[r0 + h * 128:r0 + (h + 1) * 128, q * 2048:(q + 1) * 2048], writes=[pre[i]])
        for q in range(4):
            k.dma("sync", ZS[:, q * 16:(q + 1) * 16, :], Thv[:, q * 16:(q + 1) * 16, C_GZ + h * 128:C_GZ + (h + 1) * 128], writes=[ZS])
        for q in range(4):
            k.op("act", lambda e: e.activation(ZS[:, q * 16:(q + 1) * 16, :], ZS[:, q * 16:(q + 1) * 16, :], AF.Silu), reads=[ZS], writes=[ZS])
        if DBG.get("p2stop") == 1:
            k.barrier(); P.close(); return
        for blk in range(DBG.get("nblk", S // 512)):
            o = blk * 512
            for i in range(3):
                acc = acc_r.next()
                ci = i * 4 + h
                k.op("dve", lambda e: e.tensor_scalar(acc[:], pre[i][:, 3 + o:3 + o + 512], convw[:, ci, 3:4], None, ALU.mult), reads=[pre[i], convw], writes=[acc])
                for tap in (2, 1, 0):
                    k.op("dve", lambda e: e.scalar_tensor_tensor(acc[:], pre[i][:, tap + o:tap + o + 512], convw[:, ci, tap:tap + 1], acc[:], ALU.mult, ALU.add),
                         reads=[pre[i], convw, acc], writes=[acc])
                if i == 2:
                    k.op("act", lambda e: e.activation(post[2][:, o:o + 512], acc[:], AF.Silu), reads=[acc], writes=[post[2]])
                    continue
                sl, sq, rn = sl_r.next(), sq_r.next(), rn_r.next()
                k.op("act", lambda e: e.activation(sl[:], acc[:], AF.Silu), reads=[acc], writes=[sl])
                k.op("act", lambda e: e.activation(sq[:], sl[:], AF.Square), reads=[sl], writes=[sq])
                k.op("pe", lambda e: e.matmul(pBig[:], onesb[:], sq[:], start=True, stop=True), reads=[onesb, sq], writes=[pBig])
                k.op("dve", lambda e: e.tensor_scalar(rn[:], pBig[:], 1e-6, None, ALU.add), reads=[pBig], writes=[rn])
                rsqrt_inplace(k, rn, rn[:])
                sc_ = (128.0 ** -0.5) if i == 0 else 1.0
                k.op("dve", lambda e: e.scalar_tensor_tensor(post[i][:, o:o + 512], sl[:], sc_, rn[:], ALU.mult, ALU.mult), reads=[sl, rn], writes=[post[i]])
        qT, kT, vT = post
        if DBG.get("p2stop") == 2:
            k.barrier(); P.close(); return
        k.op("pool", lambda e: e.memset(Sst[:], 0.0), writes=[Sst])
        k.op("pool", lambda e: e.memset(Sbf[:], 0.0), writes=[Sbf])
        for c in range(DBG.get("nch", NT)):
            c0 = c * 128
            cs = slice(c0, c0 + 128)
            g_col, b_col, nb_col = G[:, c, h:h + 1], BETA[:, c, h:h + 1], NBETA[:, c, h:h + 1]
            Gm, E = Gm_r.next(), E_r.next()
            k.op("dve", lambda e: e.tensor_scalar(Gm[:], UTI, g_col, None, ALU.mult), reads=[CM, G], writes=[Gm])
            k.op("pe", lambda e: e.matmul(pA[:, 0:128], LTS, Gm[:], start=True, stop=True), reads=[CM, Gm], writes=[pA])
            k.op("pe", lambda e: e.matmul(pA[:, 128:256], ONES, Gm[:], start=True, stop=True), reads=[CM, Gm], writes=[pA])
            k.op("act", lambda e: e.activation(E[:], pA[:], AF.Exp), reads=[pA], writes=[E])
            if DBG.get("cstop") == 1:
                break
            DTc, DTs, Y = DTc_r.next(), DTs_r.next(), Y_r.next()
            k.op("pool", lambda e: e.tensor_tensor(DTc[:], E[:, 0:128], UTI, ALU.mult), reads=[E, CM], writes=[DTc])
            k.op("pool", lambda e: e.tensor_tensor(DTs[:], E[:, 0:128], UTS, ALU.mult), reads=[E, CM], writes=[DTs])
            col = col_r.next()
            k.op("dve", lambda e: e.scalar_tensor_tensor(junk[:], E[:, 128:256], 1.0, IDENT, ALU.mult, ALU.mult, accum_out=col[:, 0:1]),
                 reads=[E, CM], writes=[junk, col])
            if DBG.get("cstop") == 2:
                break
            k.op("pe", lambda e: e.transpose(pT[:, 0:128], kT[:, cs], identb[:]), reads=[kT, identb], writes=[pT])
            k.op("pe", lambda e: e.transpose(pT[:, 128:256], vT[:, cs], identb[:]), reads=[vT, identb], writes=[pT])
            kg, kt, vt = kg_r.next(), kt_r.next(), vt_r.next()
            k.op("act", lambda e: e.activation(kg[:], pT[:, 0:128], AF.Copy, scale=col[:, 0:1]), reads=[pT, col], writes=[kg])
            k.op("act", lambda e: e.activation(kt[:], pT[:, 0:128], AF.Copy, scale=E[:, 127:128]), reads=[pT, E], writes=[kt])
            k.op("dve", lambda e: e.tensor_copy(vt[:], pT[:, 128:256]), reads=[pT], writes=[vt])
            if DBG.get("cstop") == 3:
                break
            k.op("pe", lambda e: e.matmul(pK[:, 0:128], kT[:, cs], kT[:, cs], start=True, stop=True), reads=[kT], writes=[pK])
            k.op("pe", lambda e: e.matmul(pK[:, 128:256], kT[:, cs], qT[:, cs], start=True, stop=True), reads=[kT, qT], writes=[pK])
            P2 = P2_r.next()
            k.op("dve", lambda e: e.scalar_tensor_tensor(Y[:], pK[:, 0:128], b_col, DTs[:], ALU.mult, ALU.mult), reads=[pK, BETA, DTs], writes=[Y])
            k.op("dve", lambda e: e.tensor_tensor(P2[:], pK[:, 128:256], DTc[:], ALU.mult), reads=[pK, DTc], writes=[P2])
            if DBG.get("cstop") == 4:
                break
            pI = pI_r.next()
            Z = Z_r.next()
            k.op("pe", lambda e: e.matmul(pI[:], Y[:], IDENT, start=True, stop=True), reads=[Y, CM], writes=[pI])
            k.op("dve", lambda e: e.tensor_copy(Z[:], pI[:]), reads=[pI], writes=[Z])
            W = Y
            Pm = Pm_r.next()
            k.op("dve", lambda e: e.tensor_tensor(Pm[:], IDENT, Y[:], ALU.subtract), reads=[CM, Y], writes=[Pm])
            for lev in range(1, 7):
                if lev > DBG.get('nlev', 6):
                    break
                Wn = Zn = None
                if lev < 6:
                    pW = pI_r.next()
                    Wn = W_r.next()
                    k.op("pe", lambda e: e.matmul(pW[:], Z[:], W[:], start=True, stop=True), reads=[Z, W], writes=[pW])
                    k.op("dve", lambda e: e.tensor_copy(Wn[:], pW[:]), reads=[pW], writes=[Wn])
                pZ = pI_r.next()
                IZ = IZ_r.next()
                k.op("pe", lambda e: e.matmul(pZ[:], W[:], Z[:], start=True, stop=True), reads=[Z, W], writes=[pZ])
                k.op("dve", lambda e: e.tensor_tensor(IZ[:], pZ[:], IDENT, ALU.add), reads=[pZ, CM], writes=[IZ])
                if lev < 6:
                    Zn = Z_r.next()
                    k.op("dve", lambda e: e.tensor_copy(Zn[:], pZ[:]), reads=[pZ], writes=[Zn])
                pP = pI_r.next()
                k.op("pe", lambda e: e.matmul(pP[:], IZ[:], Pm[:], start=True, stop=True), reads=[IZ, Pm], writes=[pP])
                if lev < 6:
                    Pn = Pm_r.next()
                    k.op("dve", lambda e: e.tensor_copy(Pn[:], pP[:]), reads=[pP], writes=[Pn])
                    Pm = Pn
                    W, Z = Wn, Zn
                else:
                    T2 = T2_r.next()
                    k.op("dve", lambda e: e.tensor_copy(T2[:], pP[:]), reads=[pP], writes=[T2])
            if DBG.get("cstop") == 5:
                break
            u, wT, qd, vn = u_r.next(), wT_r.next(), qd_r.next(), vn_r.next()
            pU = pI_r.next()
            k.op("pe", lambda e: e.matmul(pU[:], T2[:], vt[:], start=True, stop=True), reads=[T2, vt], writes=[pU])
            k.op("act", lambda e: e.activation(u[:], pU[:], AF.Copy, scale=b_col), reads=[pU, BETA], writes=[u])
            pWt = pI_r.next()
            k.op("pe", lambda e: e.matmul(pWt[:], kg[:], T2[:], start=True, stop=True), reads=[T2, kg], writes=[pWt])
            k.op("act", lambda e: e.activation(wT[:], pWt[:], AF.Copy), reads=[pWt], writes=[wT])
            k.op("pool", lambda e: e.tensor_tensor(qd[:], qT[:, cs], E[:, 128:256], ALU.mult), reads=[qT, E], writes=[qd])
            if DBG.get("cstop") == 6:
                break
            k.op("pe", lambda e: e.matmul(pS1[:], wT[:], Sbf[:], start=True, stop=True), reads=[wT, Sbf], writes=[pS1])
            k.op("dve", lambda e: e.scalar_tensor_tensor(vn[:], pS1[:], nb_col, u[:], ALU.mult, ALU.add), reads=[pS1, NBETA, u], writes=[vn])
            k.op("pe", lambda e: e.matmul(pBig[:, 0:128], qd[:], Sbf[:], start=True, stop=False), reads=[qd, Sbf], writes=[pBig])
            k.op("pe", lambda e: e.matmul(pBig[:, 0:128], P2[:], vn[:], start=False, stop=True), reads=[P2, vn], writes=[pBig])
            k.op("pe", lambda e: e.matmul(pS2[:], kt[:], vn[:], start=True, stop=True), reads=[kt, vn], writes=[pS2])
            k.op("dve", lambda e: e.scalar_tensor_tensor(Sst[:], Sst[:], E[:, 255:256], pS2[:], ALU.mult, ALU.add), reads=[Sst, E, pS2], writes=[Sst])
            k.op("act", lambda e: e.activation(Sbf[:], Sst[:], AF.Copy), reads=[Sst], writes=[Sbf])
            if DBG.get("cstop") == 7:
                break
            k.op("act", lambda e: e.activation(junk[:], pBig[:, 0:128], AF.Square, accum_out=col[:, 1:2]), reads=[pBig], writes=[junk, col])
            k.op("dve", lambda e: e.tensor_scalar(col[:, 2:3], col[:, 1:2], 1.0 / 128, 1e-6, ALU.mult, ALU.add), reads=[col], writes=[col])
            rsqrt_inplace(k, col, col[:, 2:3])
            gz, ych = gz_r.next(), y_r.next()
            k.op("pool", lambda e: e.tensor_tensor(gz[:], ZS[:, c, :], normg[:], ALU.mult), reads=[ZS, normg], writes=[gz])
            k.op("dve", lambda e: e.scalar_tensor_tensor(ych[:], pBig[:, 0:128], col[:, 2:3], gz[:], ALU.mult, ALU.mult), reads=[pBig, col, gz], writes=[ych])
            k.dma("sync", T["Y"][c0:c0 + 128, h * 128:(h + 1) * 128], ych[:], reads=[ych])
    k.new_epoch()
    P.close()


def phase3(k, nc, T, L):
    P = Pool(nc)
    CM, identb, onesb = load_consts(k, P, T)
    IDENT, UTI, LTS, UTS, ONES, NEGM, SEL = (CM[:, i, :] for i in range(7))
    gb = P.sb([128, 8], F32)
    k.dma("sync", gb[:], T["mlstm_gate_bias"][L].partition_broadcast(128), writes=[gb])
    normg = P.sb([128, 512], F32)
    k.dma("sync", normg[:], T["mlstm_norm"][L].partition_broadcast(128), writes=[normg])
    TS = load_ts(k, P, T)
    IL = P.sb([128, NT, 4], F32, "IL")
    FL = P.sb([128, NT, 4], F32, "FL")
    for h in range(4):
        k.op("dve", lambda e: e.tensor_scalar(IL[:, :, h], TS[:, :, 8 + h], gb[:, h:h + 1], None, ALU.add), reads=[TS, gb], writes=[IL])
        k.op("dve", lambda e: e.tensor_scalar(FL[:, :, h], TS[:, :, 12 + h], gb[:, 4 + h:5 + h], None, ALU.add), reads=[TS, gb], writes=[FL])
    for B_ in (IL, FL):
        k.op("act", lambda e: e.activation(B_[:], B_[:], AF.Tanh, scale=1.0 / 15.0), reads=[B_], writes=[B_])
        k.op("dve", lambda e: e.tensor_scalar(B_[:], B_[:], 15.0, None, ALU.mult), reads=[B_], writes=[B_])
    k.op("act", lambda e: e.activation(FL[:], FL[:], AF.Exp, scale=-1.0), reads=[FL], writes=[FL])
    k.op("dve", lambda e: e.tensor_scalar(FL[:], FL[:], 1.0, None, ALU.add), reads=[FL], writes=[FL])
    k.op("act", lambda e: e.activation(FL[:], FL[:], AF.Ln), reads=[FL], writes=[FL])
    k.op("dve", lambda e: e.tensor_scalar(FL[:], FL[:], -1.0, None, ALU.mult), reads=[FL], writes=[FL])

    qT = P.sb([128, S], BF16, "qT")
    kT = P.sb([128, S], BF16, "kT")
    V1 = P.sb([128, NT, 130], BF16, "V1")
    OS = P.sb([128, NT, 128], BF16, "OS")
    k.op("pool", lambda e: e.memset(V1[:], 1.0), writes=[V1])
    pA = P.ps([128, 256], F32, "pA")
    pB = P.ps([128, 128], F32, "pB")
    pK = P.ps([128, 128], F32, "pK")
    pT = P.ps([128, 256], BF16, "pT")
    pNA = P.ps([128, 130], F32, "pNA")
    pNB = P.ps([128, 130], F32, "pNB")
    pC = P.ps([128, 130], F32, "pC")

    def f32t(n, name, w=128):
        return Rot([P.sb([128, w], F32, name) for _ in range(n)])

    def bft(n, name):
        return Rot([P.sb([128, 128], BF16, name) for _ in range(n)])

    Fm_r, Di_r, ld_r, E_r = f32t(2, "Fm"), f32t(2, "Di"), f32t(2, "ld"), f32t(2, "E")
    NB_r, comb_r, hh_r = f32t(2, "NB", 130), f32t(2, "comb", 130), f32t(2, "hh")
    Sb_r, ST_r, kk_r, go_r, y_r = (bft(2, n) for n in ("Sb", "ST", "kk", "go", "ych"))
    col_r = Rot([P.sb([128, 16], F32, "col") for _ in range(3)])
    junk = P.sb([128, 128], F32, "junk")
    Cn = P.sb([128, 130], F32, "Cn")
    Cnb = P.sb([128, 130], BF16, "Cnb")
    mprev = P.sb([128, 1], F32, "mprev")
    Thv = T["Th"].rearrange("(c p) d -> p c d", p=128)
    for h in range(DBG.get("nh", 4)):
        for q in range(4):
            cs = slice(q * 2048, (q + 1) * 2048)
            k.dma("sync", qT[:, cs], T["F"][R_MQ + h * 128:R_MQ + (h + 1) * 128, cs], writes=[qT])
            k.dma("sync", kT[:, cs], T["F"][R_MK + h * 128:R_MK + (h + 1) * 128, cs], writes=[kT])
            k.dma("sync", V1[:, q * 16:(q + 1) * 16, 0:128], Thv[:, q * 16:(q + 1) * 16, C_MV + h * 128:C_MV + (h + 1) * 128], writes=[V1])
            k.dma("sync", OS[:, q * 16:(q + 1) * 16, :], Thv[:, q * 16:(q + 1) * 16, C_MO + h * 128:C_MO + (h + 1) * 128], writes=[OS])
        for q in range(4):
            cs = slice(q * 2048, (q + 1) * 2048)
            k.op("act", lambda e: e.activation(kT[:, cs], kT[:, cs], AF.Copy, scale=128.0 ** -0.5), reads=[kT], writes=[kT])
            k.op("act", lambda e: e.activation(OS[:, q * 16:(q + 1) * 16, :], OS[:, q * 16:(q + 1) * 16, :], AF.Sigmoid), reads=[OS], writes=[OS])
        k.op("pool", lambda e: e.memset(Cn[:], 0.0), writes=[Cn])
        k.op("pool", lambda e: e.memset(Cnb[:], 0.0), writes=[Cnb])
        k.op("pool", lambda e: e.memset(mprev[:], 0.0), writes=[mprev])
        for c in range(DBG.get("nch", NT)):
            c0 = c * 128
            cs = slice(c0, c0 + 128)
            f_col, i_col = FL[:, c, h:h + 1], IL[:, c, h:h + 1]
            Fm, Di, ld, E = Fm_r.next(), Di_r.next(), ld_r.next(), E_r.next()
            col = col_r.next()
            k.op("dve", lambda e: e.tensor_scalar(Fm[:], UTI, f_col, None, ALU.mult), reads=[CM, FL], writes=[Fm])
            k.op("dve", lambda e: e.tensor_scalar(Di[:], IDENT, i_col, None, ALU.mult), reads=[CM, IL], writes=[Di])
            k.op("pe", lambda e: e.matmul(pA[:, 0:128], Fm[:], LTS, start=True, stop=False), reads=[Fm, CM], writes=[pA])
            k.op("pe", lambda e: e.matmul(pA[:, 0:128], ONES, Di[:], start=False, stop=True), reads=[Di, CM], writes=[pA])
            k.op("pe", lambda e: e.matmul(pA[:, 128:256], ONES, Fm[:], start=True, stop=True), reads=[Fm, CM], writes=[pA])
            k.op("dve", lambda e: e.tensor_tensor(ld[:], pA[:, 0:128], NEGM, ALU.add), reads=[pA, CM], writes=[ld])
            k.op("dve", lambda e: e.scalar_tensor_tensor(junk[:], pA[:, 128:256], 1.0, IDENT, ALU.mult, ALU.mult, accum_out=col[:, 0:1]), reads=[pA, CM], writes=[junk, col])
            k.op("act", lambda e: e.activation(col[:, 1:2], pA[:, 255:256], AF.Copy), reads=[pA], writes=[col])
            k.op("dve", lambda e: e.reduce_max(col[:, 2:3], ld[:], AX.X), reads=[ld], writes=[col])
            k.op("pe", lambda e: e.matmul(pB[:], SEL, ld[:], start=True, stop=True), reads=[ld, CM], writes=[pB])
            k.op("dve", lambda e: e.reduce_max(col[:, 3:4], pB[:], AX.X), reads=[pB], writes=[col])
            k.op("dve", lambda e: e.scalar_tensor_tensor(col[:, 4:5], col[:, 0:1], mprev[:, 0:1], col[:, 2:3], ALU.add, ALU.max), reads=[col, mprev], writes=[col])
            k.op("dve", lambda e: e.tensor_scalar(col[:, 5:6], col[:, 4:5], -1.0, None, ALU.mult), reads=[col], writes=[col])
            k.op("dve", lambda e: e.scalar_tensor_tensor(col[:, 6:7], col[:, 0:1], mprev[:, 0:1], col[:, 5:6], ALU.add, ALU.add), reads=[col, mprev], writes=[col])
            k.op("act", lambda e: e.activation(col[:, 6:7], col[:, 6:7], AF.Exp), reads=[col], writes=[col])
            k.op("act", lambda e: e.activation(E[:], ld[:], AF.Exp, bias=col[:, 5:6]), reads=[ld, col], writes=[E])
            k.op("pe", lambda e: e.matmul(pK[:], qT[:, cs], kT[:, cs], start=True, stop=True), reads=[qT, kT], writes=[pK])
            Sb, ST = Sb_r.next(), ST_r.next()
            k.op("dve", lambda e: e.tensor_tensor(Sb[:], pK[:], E[:], ALU.mult), reads=[pK, E], writes=[Sb])
            k.op("pe", lambda e: e.transpose(pT[:, 0:128], Sb[:], identb[:]), reads=[Sb, identb], writes=[pT])
            k.op("act", lambda e: e.activation(ST[:], pT[:, 0:128], AF.Copy), reads=[pT], writes=[ST])
            k.op("pe", lambda e: e.matmul(pNA[:, 0:130], qT[:, cs], Cnb[:, 0:130], start=True, stop=True), reads=[qT, Cnb], writes=[pNA])
            k.op("pe", lambda e: e.matmul(pNB[:, 0:130], ST[:], V1[:, c, 0:130], start=True, stop=True), reads=[ST, V1], writes=[pNB])
            NB, comb, hh = NB_r.next(), comb_r.next(), hh_r.next()
            k.op("act", lambda e: e.activation(NB[:, 0:130], pNB[:, 0:130], AF.Copy), reads=[pNB], writes=[NB])
            k.op("dve", lambda e: e.scalar_tensor_tensor(comb[:, 0:130], pNA[:, 0:130], col[:, 6:7], NB[:, 0:130], ALU.mult, ALU.add), reads=[pNA, col, NB], writes=[comb])
            k.op("act", lambda e: e.activation(col[:, 7:8], comb[:, 128:129], AF.Abs), reads=[comb], writes=[col])
            k.op("act", lambda e: e.activation(col[:, 8:9], col[:, 5:6], AF.Exp), reads=[col], writes=[col])
            k.op("dve", lambda e: e.tensor_tensor(col[:, 9:10], col[:, 7:8], col[:, 8:9], ALU.max), reads=[col], writes=[col])
            k.op("dve", lambda e: e.reciprocal(col[:, 9:10], col[:, 9:10]), reads=[col], writes=[col])
            k.op("dve", lambda e: e.tensor_scalar(hh[:], comb[:, 0:128], col[:, 9:10], None, ALU.mult), reads=[comb, col], writes=[hh])
            k.op("act", lambda e: e.activation(junk[:], hh[:], AF.Square, accum_out=col[:, 10:11]), reads=[hh], writes=[junk, col])
            k.op("dve", lambda e: e.tensor_scalar(col[:, 11:12], col[:, 10:11], 1.0 / 128, 1e-6, ALU.mult, ALU.add), reads=[col], writes=[col])
            rsqrt_inplace(k, col, col[:, 11:12])
            go, ych = go_r.next(), y_r.next()
            k.op("pool", lambda e: e.tensor_tensor(go[:], OS[:, c, :], normg[:, h * 128:(h + 1) * 128], ALU.mult), reads=[OS, normg], writes=[go])
            k.op("dve", lambda e: e.scalar_tensor_tensor(ych[:], hh[:], col[:, 11:12], go[:], ALU.mult, ALU.mult), reads=[hh, col, go], writes=[ych])
            k.dma("sync", T["Y"][c0:c0 + 128, 512 + h * 128:512 + (h + 1) * 128], ych[:], reads=[ych])
            k.op("dve", lambda e: e.scalar_tensor_tensor(col[:, 12:13], col[:, 1:2], mprev[:, 0:1], col[:, 3:4], ALU.add, ALU.max), reads=[col, mprev], writes=[col])
            k.op("dve", lambda e: e.tensor_scalar(col[:, 13:14], col[:, 12:13], -1.0, None, ALU.mult), reads=[col], writes=[col])
            k.op("dve", lambda e: e.scalar_tensor_tensor(col[:, 14:15], col[:, 1:2], mprev[:, 0:1], col[:, 13:14], ALU.add, ALU.add), reads=[col, mprev], writes=[col])
            k.op("act", lambda e: e.activation(col[:, 14:15], col[:, 14:15], AF.Exp), reads=[col], writes=[col])
            k.op("dve", lambda e: e.scalar_tensor_tensor(col[:, 15:16], col[:, 0:1], -1.0, col[:, 1:2], ALU.mult, ALU.add), reads=[col], writes=[col])
            k.op("dve", lambda e: e.tensor_tensor(col[:, 15:16], col[:, 15:16], i_col, ALU.add), reads=[col, IL], writes=[col])
            k.op("act", lambda e: e.activation(col[:, 15:16], col[:, 15:16], AF.Exp, bias=col[:, 13:14]), reads=[col], writes=[col])
            kk = kk_r.next()
            k.op("pe", lambda e: e.transpose(pT[:, 128:256], kT[:, cs], identb[:]), reads=[kT, identb], writes=[pT])
            k.op("act", lambda e: e.activation(kk[:], pT[:, 128:256], AF.Copy, scale=col[:, 15:16]), reads=[pT, col], writes=[kk])
            k.op("pe", lambda e: e.matmul(pC[:, 0:130], kk[:], V1[:, c, 0:130], start=True, stop=True), reads=[kk, V1], writes=[pC])
            k.op("dve", lambda e: e.scalar_tensor_tensor(Cn[:, 0:130], Cn[:, 0:130], col[:, 14:15], pC[:, 0:130], ALU.mult, ALU.add), reads=[Cn, col, pC], writes=[Cn])
            k.op("act", lambda e: e.activation(Cnb[:, 0:130], Cn[:, 0:130], AF.Copy), reads=[Cn], writes=[Cnb])
            k.op("dve", lambda e: e.tensor_copy(mprev[:], col[:, 12:13]), reads=[col], writes=[mprev])
    k.new_epoch()
    P.close()


def phase4(k, nc, T, L):
    P = Pool(nc)
    SC = 192.0 ** -0.5
    onesb = P.sb([128, 128], BF16, "onesb")
    k.dma("gp", onesb[:], T["c_mats"][:, 4, :], writes=[onesb])
    wuq = P.sb([128, 3, 1024], BF16, "wuq")
    wukv = P.sb([128, 2, 1024], BF16, "wukv")
    for c in range(3):
        k.dma("gp", wuq[:, c, :], T["wuq"][L, c * 128:(c + 1) * 128, :], writes=[wuq])
    for c in range(2):
        k.dma("gp", wukv[:, c, :], T["mla_w_ukv"][L, c * 128:(c + 1) * 128, :], writes=[wukv])
    qg = P.sb([128, 3], F32)
    kvg = P.sb([128, 2], F32)
    rc = P.sb([64, 3], F32)
    k.dma("sync", qg[:], T["qnormT"][L], writes=[qg])
    k.dma("sync", kvg[:], T["kvnormT"][L], writes=[kvg])
    k.dma("sync", rc[:], T["c_rope"][:, :], writes=[rc])
    pS_r = Rot([P.ps([128, 512], F32, "pS") for _ in range(2)])
    pO = [P.ps([128, 512], F32, "pO") for _ in range(4)]
    with contextlib.ExitStack() as stA:
        PA = Pool(nc)
        stA.callback(PA.close)
        cq_r = Rot([PA.sb([128, 3, 512], BF16, "cq") for _ in range(2)])
        ckv_r = Rot([PA.sb([128, 2, 512], BF16, "ckv") for _ in range(2)])
        cqn_r = Rot([PA.sb([128, 3, 512], BF16, "cqn") for _ in range(2)])
        ckvn_r = Rot([PA.sb([128, 2, 512], BF16, "ckvn") for _ in range(2)])
        kr_r = Rot([PA.sb([64, 2, 512], BF16, "kr") for _ in range(2)])
        sq_r = Rot([PA.sb([128, 512], BF16, "sq") for _ in range(3)])
        rn_r = Rot([PA.sb([128, 512], F32, "rn") for _ in range(2)])
        posi_r = Rot([PA.sb([64, 512], I32, "posi") for _ in range(2)])
        ang_r = Rot([PA.sb([64, 512], F32, "ang") for _ in range(2)])
        cs_r = Rot([PA.sb([64, 2, 512], F32, "cs") for _ in range(2)])
        t_r = Rot([PA.sb([64, 512], F32, "t64") for _ in range(4)])
        st128_r = Rot([PA.sb([128, 512], BF16, "st128") for _ in range(4)])
        st64_r = Rot([PA.sb([64, 512], BF16, "st64") for _ in range(4)])
        stv_r = Rot([PA.sb([128, 512], BF16, "stv") for _ in range(2)])
        Fv = T["F"]
        for ti in range(DBG.get('nqi', S // 512)):
            ts_ = slice(ti * 512, (ti + 1) * 512)
            cq, ckv, cqn, ckvn, kr = cq_r.next(), ckv_r.next(), cqn_r.next(), ckvn_r.next(), kr_r.next()
            for c in range(3):
                k.dma("sync", cq[:, c, :], Fv[R_CQ + c * 128:R_CQ + (c + 1) * 128, ts_], writes=[cq])
            for c in range(2):
                k.dma("sync", ckv[:, c, :], Fv[R_CKV + c * 128:R_CKV + (c + 1) * 128, ts_], writes=[ckv])
            k.dma("sync", kr[:, 0, :], Fv[R_KR:R_KR + 64, ts_], writes=[kr])
            k.dma("sync", kr[:, 1, :], Fv[R_KRS:R_KRS + 64, ts_], writes=[kr])
            for (src, dst, nch, gcol, dim) in ((cq, cqn, 3, qg, 384.0), (ckv, ckvn, 2, kvg, 256.0)):
                pp = pS_r.next()
                for c in range(nch):
                    sq = sq_r.next()
                    k.op("act", lambda e: e.activation(sq[:], src[:, c, :], AF.Square), reads=[src], writes=[sq])
                    k.op("pe", lambda e: e.matmul(pp[:], onesb[:], sq[:], start=(c == 0), stop=(c == nch - 1)), reads=[onesb, sq], writes=[pp])
                rn = rn_r.next()
                k.op("dve", lambda e: e.tensor_scalar(rn[:], pp[:], 1.0 / dim, 1e-6, ALU.mult, ALU.add), reads=[pp], writes=[rn])
                rsqrt_inplace(k, rn, rn[:])
                for c in range(nch):
                    k.op("dve", lambda e: e.scalar_tensor_tensor(dst[:, c, :], src[:, c, :], gcol[:, c:c + 1], rn[:], ALU.mult, ALU.mult),
                         reads=[src, gcol, rn], writes=[dst])
            posi, ang, cs = posi_r.next(), ang_r.next(), cs_r.next()
            k.dma("sync", posi[:], T["positions"][ts_].partition_broadcast(64), writes=[posi])
            k.op("dve", lambda e: e.tensor_copy(ang[:], posi[:]), reads=[posi], writes=[ang])
            k.op("dve", lambda e: e.tensor_scalar(ang[:], ang[:], rc[:, 0:1], None, ALU.mult), reads=[ang, rc], writes=[ang])
            for j in range(2):
                uu, uf = t_r.next(), t_r.next()
                k.op("dve", lambda e: e.tensor_scalar(uu[:], ang[:], rc[:, 1 + j:2 + j], 1.0 / (2.0 * math.pi), ALU.add, ALU.mult), reads=[ang, rc], writes=[uu])
                k.op("dve", lambda e: e.tensor_copy(posi[:], uu[:]), reads=[uu], writes=[posi])
                k.op("dve", lambda e: e.tensor_copy(uf[:], posi[:]), reads=[posi], writes=[uf])
                k.op("dve", lambda e: e.tensor_tensor(uu[:], uu[:], uf[:], ALU.subtract), reads=[uu, uf], writes=[uu])
                k.op("act", lambda e: e.activation(cs[:, j, :], uu[:], AF.Sin, scale=2.0 * math.pi), reads=[uu], writes=[cs])
            t1, t2 = t_r.next(), t_r.next()
            k.op("dve", lambda e: e.tensor_tensor(t1[:], kr[:, 0, :], cs[:, 0, :], ALU.mult), reads=[kr, cs], writes=[t1])
            k.op("dve", lambda e: e.tensor_tensor(t2[:], kr[:, 1, :], cs[:, 1, :], ALU.mult), reads=[kr, cs], writes=[t2])
            st = st64_r.next()
            k.op("dve", lambda e: e.tensor_tensor(st[:], t1[:], t2[:], ALU.add), reads=[t1, t2], writes=[st])
            k.dma("sync", T["MKR"][:, ts_], st[:], reads=[st])
            for h in range(4):
                w0 = h * 256
                pp = pS_r.next()
                for c in range(3):
                    k.op("pe", lambda e: e.matmul(pp[:], wuq[:, c, w0:w0 + 128], cqn[:, c, :], start=(c == 0), stop=(c == 2)), reads=[wuq, cqn], writes=[pp])
                st = st128_r.next()
                k.op("act", lambda e: e.activation(st[:], pp[:], AF.Copy, scale=SC), reads=[pp], writes=[st])
                k.dma("sync", T["MQ"][h, 0:128, ts_], st[:], reads=[st])
                pp = pS_r.next()
                for c in range(3):
                    k.op("pe", lambda e: e.matmul(pp[0:64, :], wuq[:, c, w0 + 128:w0 + 192], cqn[:, c, :], start=(c == 0), stop=(c == 2)), reads=[wuq, cqn], writes=[pp])
                pp2 = pS_r.next()
                for c in range(3):
                    k.op("pe", lambda e: e.matmul(pp2[0:64, :], wuq[:, c, w0 + 192:w0 + 256], cqn[:, c, :], start=(c == 0), stop=(c == 2)), reads=[wuq, cqn], writes=[pp2])
                t1, t2 = t_r.next(), t_r.next()
                k.op("dve", lambda e: e.tensor_tensor(t1[:], pp[0:64, :], cs[:, 0, :], ALU.mult), reads=[pp, cs], writes=[t1])
                k.op("dve", lambda e: e.tensor_tensor(t2[:], pp2[0:64, :], cs[:, 1, :], ALU.mult), reads=[pp2, cs], writes=[t2])
                st = st64_r.next()
                k.op("dve", lambda e: e.scalar_tensor_tensor(st[:], t1[:], SC, t2[:], ALU.mult, ALU.add), reads=[t1, t2], writes=[st])
                k.op("dve", lambda e: e.scalar_tensor_tensor(st[:], t2[:], SC - 1.0, st[:], ALU.mult, ALU.add), reads=[t2, st], writes=[st])
                k.dma("sync", T["MQ"][h, 128:192, ts_], st[:], reads=[st])
                pp = pS_r.next()
                for c in range(2):
                    k.op("pe", lambda e: e.matmul(pp[:], wukv[:, c, w0:w0 + 128], ckvn[:, c, :], start=(c == 0), stop=(c == 1)), reads=[wukv, ckvn], writes=[pp])
                st = st128_r.next()
                k.op("act", lambda e: e.activation(st[:], pp[:], AF.Copy), reads=[pp], writes=[st])
                k.dma("sync", T["MK"][h, :, ts_], st[:], reads=[st])
                pp = pS_r.next()
                for s_ in range(4):
                    for c in range(2):
                        k.op("pe", lambda e: e.matmul(pp[:, s_ * 128:(s_ + 1) * 128], ckvn[:, c, s_ * 128:(s_ + 1) * 128], wukv[:, c, w0 + 128:w0 + 256], start=(c == 0), stop=(c == 1)),
                             reads=[wukv, ckvn], writes=[pp])
                stv = stv_r.next()
                k.op("act", lambda e: e.activation(stv[:], pp[:], AF.Copy), reads=[pp], writes=[stv])
                k.dma("sync", T["MV"][h].rearrange("(c p) d -> p c d", p=128)[:, ti * 4:(ti + 1) * 4, :], stv[:, :].rearrange("p (c d) -> p c d", c=4), reads=[stv])
        k.barrier()
    QN = P.sb([128, S], BF16, "QN")
    QR = P.sb([64, S], BF16, "QR")
    KN = P.sb([128, S], BF16, "KN")
    KR = P.sb([64, S], BF16, "KR")
    V1 = P.sb([128, NT, 130], BF16, "V1")
    MASK = P.sb([128, 4, 512], BF16, "MASK")
    k.dma("gp", MASK[:], T["c_amask"][:, :, :], writes=[MASK])
    k.op("pool", lambda e: e.memset(V1[:], 1.0), writes=[V1])
    for q in range(4):
        cs_ = slice(q * 2048, (q + 1) * 2048)
        k.dma("sync", KR[:, cs_], T["MKR"][:, cs_], writes=[KR])
    PT_r = Rot([P.sb([128, 512], BF16, "PT") for _ in range(3)])
    yst_r = Rot([P.sb([128, 128], BF16, "yst") for _ in range(4)])
    col_r = Rot([P.sb([128, 1], F32, "rc") for _ in range(4)])
    for h in range(DBG.get('nh', 4)):
        for q in range(4):
            cs_ = slice(q * 2048, (q + 1) * 2048)
            k.dma("sync", QN[:, cs_], T["MQ"][h, 0:128, cs_], writes=[QN])
            k.dma("sync", QR[:, cs_], T["MQ"][h, 128:192, cs_], writes=[QR])
            k.dma("sync", KN[:, cs_], T["MK"][h, :, cs_], writes=[KN])
            k.dma("sync", V1[:, q * 16:(q + 1) * 16, 0:128], T["MV"][h].rearrange("(c p) d -> p c d", p=128)[:, q * 16:(q + 1) * 16, :], writes=[V1])
        for qi in range(DBG.get('nqi', S // 512)):
            qs = slice(qi * 512, (qi + 1) * 512)
            nkb = 4 * qi + 4
            for kb in range(nkb):
                ks = slice(kb * 128, (kb + 1) * 128)
                pS = pS_r.next()
                k.op("pe", lambda e: e.matmul(pS[:], KN[:, ks], QN[:, qs], start=True, stop=False), reads=[KN, QN], writes=[pS])
                k.op("pe", lambda e: e.matmul(pS[:], KR[:, ks], QR[:, qs], start=False, stop=True), reads=[KR, QR], writes=[pS])
                PT = PT_r.next()
                k.op("act", lambda e: e.activation(PT[:], pS[:], AF.Exp), reads=[pS], writes=[PT])
                j = kb - 4 * qi
                if j >= 0:
                    k.op("pool", lambda e: e.tensor_tensor(PT[:], PT[:], MASK[:, j, :], ALU.mult), reads=[PT, MASK], writes=[PT])
                for s_ in range(4):
                    if j > s_:
                        continue
                    k.op("pe", lambda e: e.matmul(pO[s_][:, 0:130], PT[:, s_ * 128:(s_ + 1) * 128], V1[:, kb, 0:130], start=(kb == 0), stop=(kb == 4 * qi + s_)),
                         reads=[PT, V1], writes=[pO[s_]])
            for s_ in range(4):
                col, yst = col_r.next(), yst_r.next()
                k.op("dve", lambda e: e.reciprocal(col[:], pO[s_][:, 128:129]), reads=[pO[s_]], writes=[col])
                k.op("act", lambda e: e.activation(yst[:], pO[s_][:, 0:128], AF.Copy, scale=col[:]), reads=[pO[s_], col], writes=[yst])
                r0 = qi * 512 + s_ * 128
                k.dma("sync", T["Y"][r0:r0 + 128, 1024 + h * 128:1024 + (h + 1) * 128], yst[:], reads=[yst])
    k.new_epoch()
    P.close()


def phase0(k, nc, T, L):
    P = Pool(nc)
    a_r = Rot([P.sb([128, 8, 512], BF16, "cva") for _ in range(3)])
    b_r = Rot([P.sb([128, 4, 1024], BF16, "cvb") for _ in range(2)])
    for e_ in range(16):
        for nm, dst in (("moe_w1", "EW1"), ("moe_w3", "EW3")):
            a = a_r.next()
            k.dma("gp", a[:], T[nm][L, e_].rearrange("(k p) n -> p k n", p=128), writes=[a])
            k.dma("sync", T[dst][e_].rearrange("(k p) n -> p k n", p=128), a[:], reads=[a])
        b = b_r.next()
        k.dma("gp", b[:], T["moe_w2"][L, e_].rearrange("(k p) n -> p k n", p=128), writes=[b])
        k.dma("sync", T["EW2"][e_].rearrange("(k p) n -> p k n", p=128), b[:], reads=[b])
    k.new_epoch()
    P.close()


def phase5(k, nc, T, L, last):
    P = Pool(nc)
    identb = P.sb([128, 128], BF16, "identb")
    k.dma("gp", identb[:], T["c_ident"][:, :], writes=[identb])
    WBR = P.sb([128, 12, 1024], BF16, "WBR")
    WOUT = P.sb([128, 8, 1024], BF16, "WOUT")
    RW = P.sb([128, 8, 16], BF16, "RW")
    for b in range(3):
        k.dma("gp", WBR[:, b * 4:(b + 1) * 4, :], T["wbr"][L, b].rearrange("(k p) n -> p k n", p=128), writes=[WBR])
    k.dma("gp", WOUT[:], T["w_out"][L].rearrange("(k p) n -> p k n", p=128), writes=[WOUT])
    k.dma("gp", RW[:], T["router_w"].rearrange("(k p) n -> p k n", p=128), writes=[RW])
    gbT = P.sb([128, 24], F32)
    k.dma("sync", gbT[:], T["gate_biasT"][L], writes=[gbT])
    lnp = {}
    for nm in ("ln1_g", "ln1_b", "ln2_g", "ln2_b"):
        lnp[nm] = P.sb([128, D], F32, nm)
        k.dma("sync", lnp[nm][:], T[nm][L].partition_broadcast(128), writes=[lnp[nm]])
    rb = P.sb([128, 16], F32)
    k.dma("sync", rb[:], T["router_bias"].partition_broadcast(128), writes=[rb])
    sc = ln_scratch(P)
    pT = P.ps([128, 1024], BF16, "pT")
    pG_r = Rot([P.ps([128, 512], F32, "pG") for _ in range(2)])
    p1 = P.ps([128, 512], F32, "p1")
    p3 = P.ps([128, 512], F32, "p3")
    pO_r = Rot([P.ps([128, 512], F32, "pO") for _ in range(2)])
    yin_r = Rot([P.sb([128, 1536], BF16, "yin") for _ in range(1)])
    yT = P.sb([128, 12, 512], BF16, "yT")
    gl_r = Rot([P.sb([128, 512], BF16, "gl") for _ in range(2)])
    macc = P.sb([128, 512], F32, "macc")
    mtmp_r = Rot([P.sb([128, 512], F32, "mtmp") for _ in range(2)])
    mT = P.sb([128, 8, 512], BF16, "mT")
    xr_r = Rot([P.sb([128, D], F32, "xr") for _ in range(1)])
    xa = P.sb([128, D], F32, "xa")
    X1 = P.sb([128, 4, D], F32, "X1")
    xb_r = Rot([P.sb([128, D], BF16, "xb") for _ in range(2)])
    x1T = P.sb([128, 8, 512], BF16, "x1T")
    ACC = P.sb([128, 4, D], F32, "ACC")
    CW = P.sb([128, 4, 16], F32, "CW")
    w1_r = Rot([P.sb([128, 8, 512], BF16, "w1") for _ in range(2)])
    w3_r = Rot([P.sb([128, 8, 512], BF16, "w3") for _ in range(2)])
    w2_r = Rot([P.sb([128, 4, 1024], BF16, "w2") for _ in range(1)])
    sl_r = Rot([P.sb([128, 512], F32, "sl") for _ in range(2)])
    hm_r = Rot([P.sb([128, 4, 512], BF16, "hm") for _ in range(1)])
    xo_r = Rot([P.sb([128, D], F32, "xo") for _ in range(1)])
    r16 = [P.sb([128, 16], F32, "r16") for _ in range(3)]
    r24 = [P.sb([128, 4, 6], F32, "r24") for _ in range(2)]
    r4 = [P.sb([128, 4], F32, "r4") for _ in range(4)]
    r1 = [P.sb([128, 1], F32, "r1") for _ in range(2)]
    pairs = [(0, 1), (0, 2), (0, 3), (1, 2), (1, 3), (2, 3)]
    dst_out = T["y"] if last else T["xres"]
    for ti in range(DBG.get('ntile', S // 512)):
        ts_ = slice(ti * 512, (ti + 1) * 512)
        for s_ in range(4):
            r0 = ti * 512 + s_ * 128
            yin = yin_r.next()
            k.dma("sync", yin[:], T["Y"][r0:r0 + 128, :], writes=[yin])
            for (c0, n) in ((0, 8), (8, 4)):
                for c in range(n):
                    k.op("pe", lambda e: e.transpose(pT[:, c * 128:(c + 1) * 128], yin[:, (c0 + c) * 128:(c0 + c + 1) * 128], identb[:]), reads=[yin, identb], writes=[pT])
                k.op("dve", lambda e: e.tensor_copy(yT[:, c0:c0 + n, s_ * 128:(s_ + 1) * 128], pT[:, 0:n * 128].rearrange("p (k t) -> p k t", k=n)), reads=[pT], writes=[yT])
        for c in range(8):
            for b in range(3):
                gl = gl_r.next()
                row = R_GATE + b * 1024 + c * 128
                k.dma("sync", gl[:], T["F"][row:row + 128, ts_], writes=[gl])
                k.op("act", lambda e: e.activation(gl[:], gl[:], AF.Sigmoid, bias=gbT[:, b * 8 + c:b * 8 + c + 1]), reads=[gl, gbT], writes=[gl])
                pp = pG_r.next()
                for kk in range(4):
                    k.op("pe", lambda e: e.matmul(pp[:], WBR[:, b * 4 + kk, c * 128:(c + 1) * 128], yT[:, b * 4 + kk, :], start=(kk == 0), stop=(kk == 3)), reads=[WBR, yT], writes=[pp])
                if b == 0:
                    k.op("dve", lambda e: e.tensor_tensor(macc[:], pp[:], gl[:], ALU.mult), reads=[pp, gl], writes=[macc])
                else:
                    mt = mtmp_r.next()
                    k.op("dve", lambda e: e.tensor_tensor(mt[:], pp[:], gl[:], ALU.mult), reads=[pp, gl], writes=[mt])
                    if b == 1:
                        k.op("pool", lambda e: e.tensor_tensor(macc[:], macc[:], mt[:], ALU.add), reads=[macc, mt], writes=[macc])
                    else:
                        k.op("pool", lambda e: e.tensor_tensor(mT[:, c, :], macc[:], mt[:], ALU.add), reads=[macc, mt], writes=[mT])
        for s_ in range(4):
            r0 = ti * 512 + s_ * 128
            xr = xr_r.next()
            k.dma("sync", xr[:], T["xres"][r0:r0 + 128, :], writes=[xr])
            for n in range(2):
                pp = pG_r.next()
                for kk in range(8):
                    k.op("pe", lambda e: e.matmul(pp[:], mT[:, kk, s_ * 128:(s_ + 1) * 128], WOUT[:, kk, n * 512:(n + 1) * 512], start=(kk == 0), stop=(kk == 7)), reads=[mT, WOUT], writes=[pp])
                k.op("dve", lambda e: e.scalar_tensor_tensor(xa[:, n * 512:(n + 1) * 512], xr[:, n * 512:(n + 1) * 512], ALPHA, pp[:], ALU.mult, ALU.add), reads=[xr, pp], writes=[xa])
            layer_norm_tile(k, P, xa, X1s(X1, s_), lnp["ln1_g"], lnp["ln1_b"], sc)
            xb = xb_r.next()
            k.op("act", lambda e: e.activation(xb[:], X1[:, s_, :], AF.Copy), reads=[X1], writes=[xb])
            for kk in range(8):
                k.op("pe", lambda e: e.transpose(pT[:, kk * 128:(kk + 1) * 128], xb[:, kk * 128:(kk + 1) * 128], identb[:]), reads=[xb, identb], writes=[pT])
            k.op("dve", lambda e: e.tensor_copy(x1T[:, :, s_ * 128:(s_ + 1) * 128], pT[:, :].rearrange("p (k t) -> p k t", k=8)), reads=[pT], writes=[x1T])
            pp = pG_r.next()
            for kk in range(8):
                k.op("pe", lambda e: e.matmul(pp[:, 0:16], x1T[:, kk, s_ * 128:(s_ + 1) * 128], RW[:, kk, :], start=(kk == 0), stop=(kk == 7)), reads=[x1T, RW], writes=[pp])
            scr_, bi, ws = r16
            k.op("act", lambda e: e.activation(scr_[:], pp[:, 0:16], AF.Sigmoid), reads=[pp], writes=[scr_])
            k.op("dve", lambda e: e.tensor_tensor(bi[:], scr_[:], rb[:], ALU.add), reads=[scr_, rb], writes=[bi])
            biv = bi[:, :].rearrange("p (g j) -> p g j", g=4)
            PS_, PM_ = r24
            for idx, (a_, b_) in enumerate(pairs):
                k.op("dve", lambda e: e.tensor_tensor(PS_[:, :, idx], biv[:, :, a_], biv[:, :, b_], ALU.add), reads=[bi], writes=[PS_])
                k.op("dve", lambda e: e.tensor_tensor(PM_[:, :, idx], biv[:, :, a_], biv[:, :, b_], ALU.min), reads=[bi], writes=[PM_])
            gs, sec, gsel, _ = r4
            k.op("dve", lambda e: e.reduce_max(gs[:], PS_[:], AX.X), reads=[PS_], writes=[gs])
            k.op("dve", lambda e: e.reduce_max(sec[:], PM_[:], AX.X), reads=[PM_], writes=[sec])
            k.op("dve", lambda e: e.reduce_max(r1[0][:], gs[:], AX.X), reads=[gs], writes=[r1[0]])
            k.op("dve", lambda e: e.tensor_scalar(gsel[:], gs[:], r1[0][:], None, ALU.is_ge), reads=[gs, r1[0]], writes=[gsel])
            wsv = ws[:, :].rearrange("p (g j) -> p g j", g=4)
            for g_ in range(4):
                k.op("dve", lambda e: e.tensor_scalar(wsv[:, g_, :], biv[:, g_, :], sec[:, g_:g_ + 1], gsel[:, g_:g_ + 1], ALU.is_ge, ALU.mult), reads=[bi, sec, gsel], writes=[ws])
            k.op("dve", lambda e: e.tensor_tensor(ws[:], ws[:], scr_[:], ALU.mult), reads=[ws, scr_], writes=[ws])
            k.op("dve", lambda e: e.reduce_sum(r1[1][:], ws[:], AX.X), reads=[ws], writes=[r1[1]])
            k.op("dve", lambda e: e.reciprocal(r1[1][:], r1[1][:]), reads=[r1[1]], writes=[r1[1]])
            k.op("dve", lambda e: e.tensor_scalar(CW[:, s_, :], ws[:], r1[1][:], None, ALU.mult), reads=[ws, r1[1]], writes=[CW])
        for e_ in range(DBG.get('nexp', 16)):
            w1, w3, w2 = w1_r.next(), w3_r.next(), w2_r.next()
            k.dma("sync", w1[:], T["EW1"][e_].rearrange("(k p) n -> p k n", p=128), writes=[w1])
            k.dma("sync", w3[:], T["EW3"][e_].rearrange("(k p) n -> p k n", p=128), writes=[w3])
            k.dma("sync", w2[:], T["EW2"][e_].rearrange("(k p) n -> p k n", p=128), writes=[w2])
            hm = hm_r.next()
            for dc in range(4):
                for kk in range(8):
                    k.op("pe", lambda e: e.matmul(p1[:], w1[:, kk, dc * 128:(dc + 1) * 128], x1T[:, kk, :], start=(kk == 0), stop=(kk == 7)), reads=[w1, x1T], writes=[p1])
                for kk in range(8):
                    k.op("pe", lambda e: e.matmul(p3[:], w3[:, kk, dc * 128:(dc + 1) * 128], x1T[:, kk, :], start=(kk == 0), stop=(kk == 7)), reads=[w3, x1T], writes=[p3])
                sl = sl_r.next()
                k.op("act", lambda e: e.activation(sl[:], p1[:], AF.Silu), reads=[p1], writes=[sl])
                k.op("dve", lambda e: e.tensor_tensor(hm[:, dc, :], sl[:], p3[:], ALU.mult), reads=[sl, p3], writes=[hm])
            for s_ in range(4):
                for n in range(2):
                    po = pO_r.next()
                    for dc in range(4):
                        k.op("pe", lambda e: e.matmul(po[:], hm[:, dc, s_ * 128:(s_ + 1) * 128], w2[:, dc, n * 512:(n + 1) * 512], start=(dc == 0), stop=(dc == 3)), reads=[hm, w2], writes=[po])
                    asl = ACC[:, s_, n * 512:(n + 1) * 512]
                    if e_ == 0:
                        k.op("dve", lambda e: e.tensor_scalar(asl, po[:], CW[:, s_, 0:1], None, ALU.mult), reads=[po, CW], writes=[ACC])
                    else:
                        k.op("dve", lambda e: e.scalar_tensor_tensor(asl, po[:], CW[:, s_, e_:e_ + 1], asl, ALU.mult, ALU.add), reads=[po, CW, ACC], writes=[ACC])
        for s_ in range(4):
            r0 = ti * 512 + s_ * 128
            k.op("dve", lambda e: e.scalar_tensor_tensor(xa[:], X1[:, s_, :], ALPHA, ACC[:, s_, :], ALU.mult, ALU.add), reads=[X1, ACC], writes=[xa])
            xo = xo_r.next()
            layer_norm_tile(k, P, xa, xo, lnp["ln2_g"], lnp["ln2_b"], sc)
            k.dma("sync", dst_out[r0:r0 + 128, :], xo[:], reads=[xo])
    k.new_epoch()
    P.close()


class X1s:
    def __init__(self, buf, s_):
        self._b = buf
        self._s = s_

    def __getitem__(self, idx):
        return self._b.ap[:, self._s, :]

    @property
    def w(self):
        return self._b.w

    @w.setter
    def w(self, v):
        self._b.w = v

    @property
    def r(self):
        return self._b.r

    @r.setter
    def r(self, v):
        self._b.r = v


def build(dbg=None, phases=("p1",), nlayers=DEPTH):
    nc = bass.Bass("TRN2", target_bir_lowering=False)
    T = {}

    def inp(name, shape, dt=F32):
        T[name] = nc.dram_tensor(name, list(shape), dt, kind="ExternalInput").ap()

    def scr(name, shape, dt):
        kind = "ExternalOutput" if (dbg and name in dbg) else "Internal"
        if name in DBG.get("ext_in", ()):
            kind = "ExternalInput"
        T[name] = nc.dram_tensor(name, list(shape), dt, kind=kind).ap()

    inp("x", [S, D])
    inp("win", [DEPTH, D, NW])
    inp("ln_in_g", [D])
    inp("ln_in_b", [D])
    inp("c_ident", [128, 128])
    inp("c_mats", [128, 7, 128])
    inp("gdn_a_log", [DEPTH, 4])
    inp("gdn_dt_bias", [DEPTH, 4])
    inp("gdn_norm", [DEPTH, 128])
    inp("gdn_convT", [DEPTH, 128, 12, 4])
    inp("mlstm_gate_bias", [DEPTH, 8])
    inp("mlstm_norm", [DEPTH, 512])
    inp("wuq", [DEPTH, 384, 1024])
    inp("mla_w_ukv", [DEPTH, 256, 1024])
    inp("qnormT", [DEPTH, 128, 3])
    inp("kvnormT", [DEPTH, 128, 2])
    inp("c_rope", [64, 3])
    inp("c_amask", [128, 4, 512])
    inp("positions", [S], I32)
    inp("wbr", [DEPTH, 3, 512, 1024])
    inp("w_out", [DEPTH, D, D])
    inp("router_w", [D, 16])
    inp("router_bias", [16])
    inp("gate_biasT", [DEPTH, 128, 24])
    for nm in ("ln1_g", "ln1_b", "ln2_g", "ln2_b"):
        inp(nm, [DEPTH, D])
    inp("moe_w1", [DEPTH, 16, D, 512])
    inp("moe_w3", [DEPTH, 16, D, 512])
    inp("moe_w2", [DEPTH, 16, 512, D])
    T["y"] = nc.dram_tensor("y", [S, D], F32, kind="ExternalOutput").ap()
    scr("xres", [S, D], F32)
    scr("F", [NFEAT, S], BF16)
    scr("Th", [S, NTOK], BF16)
    scr("Ts", [S, 16], F32)
    scr("Y", [S, 1536], BF16)
    scr("MQ", [4, 192, S], BF16)
    scr("MK", [4, 128, S], BF16)
    scr("MKR", [64, S], BF16)
    scr("MV", [4, S, 128], BF16)
    scr("EW1", [16, D, 512], BF16)
    scr("EW3", [16, D, 512], BF16)
    scr("EW2", [16, 512, D], BF16)
    k = K(nc)
    for L in range(nlayers):
        if "p1" in phases:
            phase1(k, nc, T, L)
        if "p2" in phases:
            phase2(k, nc, T, L)
        if "p3" in phases:
            phase3(k, nc, T, L)
        if "p4" in phases:
            phase4(k, nc, T, L)
        if "p5" in phases:
            phase0(k, nc, T, L)
            phase5(k, nc, T, L, last=(L == nlayers - 1))
    k.finish()
    return nc


def const_mats():
    a = np.arange(128)[:, None]
    b = np.arange(128)[None, :]
    m = np.zeros((128, 7, 128), np.float32)
    m[:, 0] = (a == b)
    m[:, 1] = (a <= b)
    m[:, 2] = (a > b)
    m[:, 3] = (a < b)
    m[:, 4] = 1.0
    m[:, 5] = np.where(a < b, -1e30, 0.0)
    m[:, 6] = (a == 127)
    return m


def perm_wuq(w):
    idx = []
    for h in range(4):
        b = h * 192
        idx += list(range(b, b + 128)) + list(range(b + 128, b + 192)) + list(range(b + 160, b + 192)) + list(range(b + 128, b + 160))
    return np.ascontiguousarray(w[:, :, np.array(idx)])


def rope_consts():
    f = np.arange(32, dtype=np.float32)
    inv = (1.0 / (np.float32(10000.0) ** (f * 2 / 64))).astype(np.float32)
    c = np.zeros((64, 3), np.float32)
    c[:, 0] = np.concatenate([inv, inv])
    c[:, 1] = math.pi / 2
    c[:32, 2] = math.pi
    c[32:, 2] = 0.0
    return c


def attn_masks():
    kk = np.arange(128)[:, None, None]
    jj = np.arange(4)[None, :, None]
    qq = np.arange(512)[None, None, :]
    return (qq >= kk + 128 * jj).astype(np.float32)


def perm_win(w_in):
    o = {}
    names = ["g_q", "g_k", "g_v", "g_z", "g_a", "g_b", "m_q", "m_k", "m_v", "m_o", "m_i", "m_f", "c_q", "c_kv", "k_rope", "gate"]
    sizes = [512, 512, 512, 512, 4, 4, 512, 512, 512, 512, 4, 4, 384, 256, 64, 3072]
    a = 0
    for n, s in zip(names, sizes):
        o[n] = np.arange(a, a + s)
        a += s
    kr = o["k_rope"]
    krs = np.concatenate([kr[32:], kr[:32]])
    idx = np.concatenate([o["g_q"], o["g_k"], o["g_v"], o["m_q"], o["m_k"], o["c_q"], o["c_kv"], kr, krs, o["gate"],
                          o["g_z"], o["m_v"], o["m_o"], o["g_a"], o["g_b"], o["m_i"], o["m_f"]])
    assert idx.size == NW
    return np.ascontiguousarray(w_in[:, :, idx])


def host_inputs(inputs):
    common = {
        "win": perm_win(np.asarray(inputs["w_in"], np.float32)),
        "ln_in_g": np.asarray(inputs["ln_in_g"], np.float32),
        "ln_in_b": np.asarray(inputs["ln_in_b"], np.float32),
        "c_ident": np.eye(128, dtype=np.float32),
        "c_mats": const_mats(),
        "gdn_a_log": np.asarray(inputs["gdn_a_log"], np.float32),
        "gdn_dt_bias": np.asarray(inputs["gdn_dt_bias"], np.float32),
        "gdn_norm": np.asarray(inputs["gdn_norm"], np.float32),
        "mlstm_gate_bias": np.asarray(inputs["mlstm_gate_bias"], np.float32),
        "mlstm_norm": np.asarray(inputs["mlstm_norm"], np.float32),
        "wuq": perm_wuq(np.asarray(inputs["mla_w_uq"], np.float32)),
        "mla_w_ukv": np.asarray(inputs["mla_w_ukv"], np.float32),
        "qnormT": np.ascontiguousarray(np.asarray(inputs["mla_q_norm"], np.float32).reshape(DEPTH, 3, 128).transpose(0, 2, 1)),
        "kvnormT": np.ascontiguousarray(np.asarray(inputs["mla_kv_norm"], np.float32).reshape(DEPTH, 2, 128).transpose(0, 2, 1)),
        "c_rope": rope_consts(),
        "c_amask": attn_masks(),
        "wbr": np.ascontiguousarray(np.stack([np.asarray(inputs[n], np.float32) for n in ("w_br_gdn", "w_br_mlstm", "w_br_mla")], axis=1)),
        "w_out": np.asarray(inputs["w_out"], np.float32),
        "router_w": np.asarray(inputs["router_w"], np.float32),
        "router_bias": np.asarray(inputs["router_bias"], np.float32),
        "gate_biasT": np.ascontiguousarray(np.asarray(inputs["gate_bias"], np.float32).reshape(DEPTH, 24, 128).transpose(0, 2, 1)),
        "ln1_g": np.asarray(inputs["ln1_g"], np.float32), "ln1_b": np.asarray(inputs["ln1_b"], np.float32),
        "ln2_g": np.asarray(inputs["ln2_g"], np.float32), "ln2_b": np.asarray(inputs["ln2_b"], np.float32),
        "moe_w1": np.asarray(inputs["moe_w1"], np.float32), "moe_w3": np.asarray(inputs["moe_w3"], np.float32),
        "moe_w2": np.asarray(inputs["moe_w2"], np.float32),
        "gdn_convT": np.ascontiguousarray(np.asarray(inputs["gdn_conv"], np.float32).reshape(DEPTH, 4, 12, 128).transpose(0, 3, 2, 1)),
    }
    maps = []
    for c in range(8):
        m = dict(common)
        m["x"] = np.ascontiguousarray(inputs["x"][c % 4])
        m["positions"] = np.ascontiguousarray(inputs["positions"][c % 4]).astype(np.int32)
        maps.append(m)
    return maps


def kernel(**inputs):
    nc = build(phases=("p1", "p2", "p3", "p4", "p5"))
    maps = host_inputs(inputs)
    res = run_bass_kernel_spmd(nc, maps, core_ids=list(range(8)))
    out = np.stack([res.results[c]["y"] for c in range(4)], axis=0)
    return out.astype(np.float32)
```

```python
import contextlib
import math
import numpy as np
import concourse.bass as bass
import concourse.mybir as mybir
from concourse.bass_utils import run_bass_kernel_spmd

F32 = mybir.dt.float32
BF16 = mybir.dt.bfloat16
I32 = mybir.dt.int32
ALU = mybir.AluOpType
AF = mybir.ActivationFunctionType
AX = mybir.AxisListType

S = 8192
D = 1024
NT = S // 128
DEPTH = 2
DBG = {}
ALPHA = (2 * DEPTH) ** 0.25
NFEAT = 6400
NTOK = 1536
NW = NFEAT + NTOK + 16
R_GQ, R_GK, R_GV, R_MQ, R_MK, R_CQ, R_CKV, R_KR, R_KRS, R_GATE = 0, 512, 1024, 1536, 2048, 2560, 2944, 3200, 3264, 3328
C_GZ, C_MV, C_MO = 0, 512, 1024


class Buf:
    __slots__ = ("ap", "w", "r", "name")

    def __init__(self, ap, name=""):
        self.ap = ap
        self.w = None
        self.r = {}
        self.name = name

    def __getitem__(self, idx):
        return self.ap[idx]


class Eng:
    def __init__(self, name, eng, sem):
        self.name = name
        self.eng = eng
        self.sem = sem
        self.n = 0
        self.seen = {}


class K:
    def __init__(self, nc, n_dma_sems=8):
        self.nc = nc
        self.stack = contextlib.ExitStack()
        self.engs = {}
        for name, eng in (("pe", nc.tensor), ("act", nc.scalar), ("dve", nc.vector), ("pool", nc.gpsimd)):
            sem = self.stack.enter_context(nc.semaphore("s_" + name))
            self.engs[name] = Eng(name, eng, sem)
        self.engs["sync"] = Eng("sync", nc.sync, None)
        self.dmaq = {}
        for qname, eng, iss in (("sync", nc.sync, "sync"), ("gp", nc.gpsimd, "pool")):
            sems = [self.stack.enter_context(nc.semaphore(f"d_{qname}{i}")) for i in range(n_dma_sems)]
            self.dmaq[qname] = dict(eng=eng, sems=sems, vals=[0] * n_dma_sems, nxt=0, issuer=iss)
        self.dma_id = 0
        self.rr = 0
        self.epoch = 0

    def _wait(self, issuer, dep):
        if dep is None:
            return
        if dep[0] == "e":
            _, ename, seq, ep = dep
            if ep < self.epoch:
                return
            if ename == issuer.name and ename == "pe":
                return
            if issuer.seen.get(ename, 0) >= seq:
                return
            issuer.seen[ename] = seq
            issuer.eng.wait_ge(self.engs[ename].sem, seq)
        else:
            _, sem, val, tok = dep
            key = ("d", tok)
            if issuer.seen.get(key):
                return
            issuer.seen[key] = True
            issuer.eng.wait_ge(sem, val)

    def _deps(self, issuer, reads, writes):
        for b in reads:
            self._wait(issuer, b.w)
        for b in writes:
            self._wait(issuer, b.w)
            for d in list(b.r.values()):
                self._wait(issuer, d)

    def _mark(self, dep, reads, writes):
        key = dep[1] if dep[0] == "e" else ("d", dep[3])
        for b in reads:
            b.r[key] = dep
        for b in writes:
            b.w = dep
            b.r = {}

    def op(self, ename, fn, reads=(), writes=()):
        e = self.engs[ename]
        self._deps(e, reads, writes)
        ins = fn(e.eng)
        e.n += 1
        ins.then_inc(e.sem, 1)
        self._mark(("e", ename, e.n, self.epoch), reads, writes)
        return ins

    def ev(self, fn, reads=(), writes=()):
        return self.op("dve", fn, reads, writes)

    def dma(self, qname, out, in_, reads=(), writes=(), **kw):
        q = self.dmaq[qname]
        issuer = self.engs[q["issuer"]]
        i = q["nxt"]
        q["nxt"] = (i + 1) % len(q["sems"])
        sem = q["sems"][i]
        if q["vals"][i] > 0:
            issuer.eng.wait_ge(sem, q["vals"][i])
        self._deps(issuer, reads, writes)
        ins = q["eng"].dma_start(out=out, in_=in_, **kw)
        q["vals"][i] += 16
        ins.then_inc(sem, 16)
        self.dma_id += 1
        dep = ("d", sem, q["vals"][i], self.dma_id)
        self._mark(dep, reads, writes)
        return dep

    def barrier(self):
        for iss in self.engs.values():
            for e in self.engs.values():
                if e.sem is not None and e.n > 0 and e is not iss:
                    if iss.seen.get(e.name, 0) < e.n:
                        iss.seen[e.name] = e.n
                        iss.eng.wait_ge(e.sem, e.n)
            for q in self.dmaq.values():
                for sem, v in zip(q["sems"], q["vals"]):
                    if v > 0:
                        iss.eng.wait_ge(sem, v)

    def new_epoch(self):
        self.barrier()
        self.epoch += 1
        for name, e in self.engs.items():
            if e.sem is not None:
                e.sem = self.stack.enter_context(self.nc.semaphore(f"s_{name}_{self.epoch}"))
                e.n = 0
            e.seen = {k_: v for k_, v in e.seen.items() if isinstance(k_, tuple)}

    def finish(self):
        self.barrier()
        self.stack.close()


class Pool:
    cnt = 0

    def __init__(self, nc):
        self.nc = nc
        self.st = contextlib.ExitStack()
        self.i = 0

    def sb(self, shape, dt, name=None):
        Pool.cnt += 1
        name = f"{name or 't'}_{Pool.cnt}"
        return Buf(self.st.enter_context(self.nc.sbuf_tensor(name, list(shape), dt)), name)

    def ps(self, shape, dt, name=None):
        Pool.cnt += 1
        name = f"{name or 'p'}_{Pool.cnt}"
        full = 2048 // (2 if dt == BF16 else 4)
        t = self.st.enter_context(self.nc.psum_tensor(name, [shape[0], full], dt))
        return Buf(t[:, 0:shape[1]], name)

    def close(self):
        self.st.close()


class Rot:
    def __init__(self, items):
        self.items = items
        self.i = 0

    def next(self):
        b = self.items[self.i % len(self.items)]
        self.i += 1
        return b


def layer_norm_tile(k, P, xin, out, gbc, bbc, sc, eps=1e-5, pre=None):
    s1, nm, ssq, rstd, junk = sc["s1"], sc["nm"], sc["ssq"], sc["rstd"], sc["junk"]
    k.op("act", lambda e: e.activation(junk[:], xin[:], AF.Copy, accum_out=s1[:]), reads=[xin], writes=[junk, s1])
    k.op("dve", lambda e: e.tensor_scalar(nm[:], s1[:], -1.0 / D, None, ALU.mult), reads=[s1], writes=[nm])
    k.op("act", lambda e: e.activation(junk[:], xin[:], AF.Square, bias=nm[:], accum_out=ssq[:]), reads=[xin, nm], writes=[junk, ssq])
    k.op("dve", lambda e: e.tensor_scalar(rstd[:], ssq[:], 1.0 / D, eps, ALU.mult, ALU.add), reads=[ssq], writes=[rstd])
    k.op("act", lambda e: e.activation(rstd[:], rstd[:], AF.Sqrt), reads=[rstd], writes=[rstd])
    k.op("dve", lambda e: e.reciprocal(rstd[:], rstd[:]), reads=[rstd], writes=[rstd])
    k.op("dve", lambda e: e.tensor_scalar(out[:], xin[:], nm[:], rstd[:], ALU.add, ALU.mult), reads=[xin, nm, rstd], writes=[out])
    k.op("pool", lambda e: e.tensor_tensor(out[:], out[:], gbc[:], ALU.mult), reads=[out, gbc], writes=[out])
    k.op("pool", lambda e: e.tensor_tensor(out[:], out[:], bbc[:], ALU.add), reads=[out, bbc], writes=[out])


def ln_scratch(P):
    return dict(s1=P.sb([128, 1], F32), nm=P.sb([128, 1], F32), ssq=P.sb([128, 1], F32),
                rstd=P.sb([128, 1], F32), junk=P.sb([128, D], F32))


def phase1(k, nc, T, L):
    P = Pool(nc)
    first = (L == 0)
    W = [P.sb([128, NW], BF16, "W") for _ in range(8)]
    for kk in range(8):
        for h0 in range(0, NW, 1988):
            k.dma("gp", W[kk][:, h0:h0 + 1988], T["win"][L, kk * 128:(kk + 1) * 128, h0:h0 + 1988], writes=[W[kk]])
    ident = P.sb([128, 128], BF16, "ident")
    k.dma("gp", ident[:], T["c_ident"][:, :], writes=[ident])
    if first:
        gbc = P.sb([128, D], F32)
        bbc = P.sb([128, D], F32)
        k.dma("sync", gbc[:], T["ln_in_g"].partition_broadcast(128), writes=[gbc])
        k.dma("sync", bbc[:], T["ln_in_b"].partition_broadcast(128), writes=[bbc])
        sc = ln_scratch(P)
        xn_r = Rot([P.sb([128, D], F32, "xn") for _ in range(2)])
    xin_r = Rot([P.sb([128, D], F32, "xin") for _ in range(2)])
    xb_r = Rot([P.sb([128, D], BF16, "xb") for _ in range(2)])
    xT_r = Rot([P.sb([128, 8, 512], BF16, "xT") for _ in range(2)])
    tp_r = Rot([P.ps([128, D], BF16, "tp") for _ in range(2)])
    pp_r = Rot([P.ps([128, 512], F32, "pp") for _ in range(4)])
    stf_r = Rot([P.sb([128, 512], BF16, "stf") for _ in range(4)])
    stt_r = Rot([P.sb([128, NTOK], BF16, "stt") for _ in range(2)])
    sts_r = Rot([P.sb([128, 16], F32, "sts") for _ in range(2)])
    src = T["x"] if first else T["xres"]
    evi = 0
    for ti in range(S // 512):
        xT = xT_r.next()
        for s in range(4):
            r0 = ti * 512 + s * 128
            xi = xin_r.next()
            k.dma("sync", xi[:], src[r0:r0 + 128, :], writes=[xi])
            if first:
                xn = xn_r.next()
                layer_norm_tile(k, P, xi, xn, gbc, bbc, sc)
                k.dma("sync", T["xres"][r0:r0 + 128, :], xn[:], reads=[xn])
                xi = xn
            xb = xb_r.next()
            k.op("act", lambda e: e.activation(xb[:], xi[:], AF.Copy), reads=[xi], writes=[xb])
            tp = tp_r.next()
            for kk in range(8):
                k.op("pe", lambda e: e.transpose(tp[:, kk * 128:(kk + 1) * 128], xb[:, kk * 128:(kk + 1) * 128], ident[:]),
                     reads=[xb, ident], writes=[tp])
            k.op("dve", lambda e: e.tensor_copy(xT[:, :, s * 128:(s + 1) * 128], tp[:, :].rearrange("p (k t) -> p k t", k=8)),
                 reads=[tp], writes=[xT])
        for c in range(NFEAT // 128):
            pp = pp_r.next()
            for kk in range(8):
                k.op("pe", lambda e: e.matmul(pp[:], W[kk][:, c * 128:(c + 1) * 128], xT[:, kk, :], start=(kk == 0), stop=(kk == 7)),
                     reads=[W[kk], xT], writes=[pp])
            stf = stf_r.next()
            evi += 1
            if evi % 2:
                k.op("act", lambda e: e.activation(stf[:], pp[:], AF.Copy), reads=[pp], writes=[stf])
            else:
                k.op("dve", lambda e: e.tensor_copy(stf[:], pp[:]), reads=[pp], writes=[stf])
            k.dma("sync", T["F"][c * 128:(c + 1) * 128, ti * 512:(ti + 1) * 512], stf[:], reads=[stf])
        for s in range(4):
            r0 = ti * 512 + s * 128
            stt = stt_r.next()
            sts = sts_r.next()
            for j in range(4):
                pp = pp_r.next()
                n = 512 if j < 3 else 16
                c0 = NFEAT + j * 512
                for kk in range(8):
                    k.op("pe", lambda e: e.matmul(pp[:, :n], xT[:, kk, s * 128:(s + 1) * 128], W[kk][:, c0:c0 + n], start=(kk == 0), stop=(kk == 7)),
                         reads=[W[kk], xT], writes=[pp])
                evi += 1
                dst = stt[:, j * 512:(j + 1) * 512] if j < 3 else sts[:]
                dbuf = stt if j < 3 else sts
                if evi % 2:
                    k.op("act", lambda e: e.activation(dst, pp[:, :n], AF.Copy), reads=[pp], writes=[dbuf])
                else:
                    k.op("dve", lambda e: e.tensor_copy(dst, pp[:, :n]), reads=[pp], writes=[dbuf])
            k.dma("sync", T["Th"][r0:r0 + 128, :], stt[:], reads=[stt])
            k.dma("sync", T["Ts"][r0:r0 + 128, :], sts[:], reads=[sts])
    k.new_epoch()
    P.close()


def load_consts(k, P, T):
    CM = P.sb([128, 7, 128], F32, "CM")
    k.dma("sync", CM[:], T["c_mats"][:, :, :], writes=[CM])
    identb = P.sb([128, 128], BF16, "identb")
    k.dma("gp", identb[:], T["c_ident"][:, :], writes=[identb])
    onesb = P.sb([128, 128], BF16, "onesb")
    k.dma("gp", onesb[:], T["c_mats"][:, 4, :], writes=[onesb])
    return CM, identb, onesb


def rsqrt_inplace(k, buf, ap):
    k.op("act", lambda e: e.activation(ap, ap, AF.Sqrt), reads=[buf], writes=[buf])
    k.op("dve", lambda e: e.reciprocal(ap, ap), reads=[buf], writes=[buf])


def load_ts(k, P, T):
    TS = P.sb([128, NT, 16], F32, "TS")
    src = T["Ts"].rearrange("(c p) j -> p c j", p=128)
    for q in range(4):
        k.dma("sync", TS[:, q * 16:(q + 1) * 16, :], src[:, q * 16:(q + 1) * 16, :], writes=[TS])
    return TS


def phase2(k, nc, T, L):
    P = Pool(nc)
    CM, identb, onesb = load_consts(k, P, T)
    IDENT, UTI, LTS, UTS, ONES = (CM[:, i, :] for i in range(5))
    alog = P.sb([128, 4], F32)
    dtb = P.sb([128, 4], F32)
    normg = P.sb([128, 128], F32)
    convw = P.sb([128, 12, 4], F32)
    k.dma("sync", alog[:], T["gdn_a_log"][L].partition_broadcast(128), writes=[alog])
    k.dma("sync", dtb[:], T["gdn_dt_bias"][L].partition_broadcast(128), writes=[dtb])
    k.dma("sync", normg[:], T["gdn_norm"][L].partition_broadcast(128), writes=[normg])
    k.dma("sync", convw[:], T["gdn_convT"][L], writes=[convw])
    negA = P.sb([128, 4], F32)
    k.op("act", lambda e: e.activation(negA[:], alog[:], AF.Exp), reads=[alog], writes=[negA])
    k.op("dve", lambda e: e.tensor_scalar(negA[:], negA[:], -1.0, None, ALU.mult), reads=[negA], writes=[negA])
    TS = load_ts(k, P, T)
    G = P.sb([128, NT, 4], F32, "G")
    BETA = P.sb([128, NT, 4], F32, "BETA")
    NBETA = P.sb([128, NT, 4], F32, "NBETA")
    t1 = P.sb([128, NT, 4], F32)
    t2 = P.sb([128, NT, 4], F32)
    for h in range(4):
        k.op("dve", lambda e: e.tensor_scalar(G[:, :, h], TS[:, :, h], dtb[:, h:h + 1], None, ALU.add), reads=[TS, dtb], writes=[G])
    k.op("act", lambda e: e.activation(t1[:], G[:], AF.Abs), reads=[G], writes=[t1])
    k.op("act", lambda e: e.activation(t1[:], t1[:], AF.Exp, scale=-1.0), reads=[t1], writes=[t1])
    k.op("dve", lambda e: e.tensor_scalar(t1[:], t1[:], 1.0, None, ALU.add), reads=[t1], writes=[t1])
    k.op("act", lambda e: e.activation(t1[:], t1[:], AF.Ln), reads=[t1], writes=[t1])
    k.op("dve", lambda e: e.scalar_tensor_tensor(t2[:], G[:], 0.0, t1[:], ALU.max, ALU.add), reads=[G, t1], writes=[t2])
    for h in range(4):
        k.op("dve", lambda e: e.tensor_scalar(G[:, :, h], t2[:, :, h], negA[:, h:h + 1], None, ALU.mult), reads=[t2, negA], writes=[G])
    k.op("act", lambda e: e.activation(BETA[:], TS[:, :, 4:8], AF.Sigmoid), reads=[TS], writes=[BETA])
    k.op("dve", lambda e: e.tensor_scalar(NBETA[:], BETA[:], -1.0, None, ALU.mult), reads=[BETA], writes=[NBETA])

    if DBG.get("p2stop") == 0:
        k.barrier(); P.close(); return
    pre = [P.sb([128, 3 + S], BF16, "pre") for _ in range(3)]
    post = [P.sb([128, S], BF16, "post") for _ in range(3)]
    ZS = P.sb([128, NT, 128], BF16, "ZS")
    for i in range(3):
        k.op("pool", lambda e: e.memset(pre[i][:, 0:3], 0.0), writes=[pre[i]])
    acc_r = Rot([P.sb([128, 512], F32, "acc") for _ in range(2)])
    sl_r = Rot([P.sb([128, 512], F32, "sl") for _ in range(2)])
    sq_r = Rot([P.sb([128, 512], BF16, "sq") for _ in range(2)])
    rn_r = Rot([P.sb([128, 512], F32, "rn") for _ in range(2)])
    pBig = P.ps([128, 512], F32, "pBig")
    pA = P.ps([128, 256], F32, "pA")
    pT = P.ps([128, 256], BF16, "pT")
    pK = P.ps([128, 256], F32, "pK")
    pI_r = Rot([P.ps([128, 128], F32, "pI") for _ in range(2)])
    pS1 = P.ps([128, 128], F32, "pS1")
    pS2 = P.ps([128, 128], F32, "pS2")

    def f32t(n, name):
        return Rot([P.sb([128, 128], F32, name) for _ in range(n)])

    def bft(n, name):
        return Rot([P.sb([128, 128], BF16, name) for _ in range(n)])

    Gm_r, E_r = f32t(2, "Gm"), Rot([P.sb([128, 256], F32, "E") for _ in range(2)])
    DTc_r, DTs_r, Y_r = f32t(2, "DTc"), f32t(2, "DTs"), f32t(2, "Y")
    W_r, Z_r, IZ_r, Pm_r = f32t(3, "W"), f32t(3, "Z"), f32t(2, "IZ"), f32t(3, "Pm")
    u_r = f32t(2, "u")
    kg_r, kt_r, vt_r, P2_r, T2_r, wT_r, qd_r, vn_r, y_r, gz_r = (bft(2, n) for n in ("kg", "kt", "vt", "P2", "T2", "wT", "qd", "vn", "ych", "gz"))
    junk = P.sb([128, 128], F32, "junk")
    col_r = Rot([P.sb([128, 4], F32, "col") for _ in range(3)])
    Sst = P.sb([128, 128], F32, "Sst")
    Sbf = P.sb([128, 128], BF16, "Sbf")
    Fsc = T["F"]
    Thv = T["Th"].rearrange("(c p) d -> p c d", p=128)
    for h in range(DBG.get("nh", 4)):
        for i, r0 in enumerate((R_GQ, R_GK, R_GV)):
            for q in range(4):
                k.dma("sync", pre[i][:, 3 + q * 2048:3 + (q + 1) * 2048], Fsc[r0 + h * 128:r0 + (h + 1) * 128, q * 2048:(q + 1) * 2048], writes=[pre[i]])
        for q in range(4):
            k.dma("sync", ZS[:, q * 16:(q + 1) * 16, :], Thv[:, q * 16:(q + 1) * 16, C_GZ + h * 128:C_GZ + (h + 1) * 128], writes=[ZS])
        for q in range(4):
            k.op("act", lambda e: e.activation(ZS[:, q * 16:(q + 1) * 16, :], ZS[:, q * 16:(q + 1) * 16, :], AF.Silu), reads=[ZS], writes=[ZS])
        if DBG.get("p2stop") == 1:
            k.barrier(); P.close(); return
        for blk in range(DBG.get("nblk", S // 512)):
            o = blk * 512
            for i in range(3):
                acc = acc_r.next()
                ci = i * 4 + h
                k.op("dve", lambda e: e.tensor_scalar(acc[:], pre[i][:, 3 + o:3 + o + 512], convw[:, ci, 3:4], None, ALU.mult), reads=[pre[i], convw], writes=[acc])
                for tap in (2, 1, 0):
                    k.op("dve", lambda e: e.scalar_tensor_tensor(acc[:], pre[i][:, tap + o:tap + o + 512], convw[:, ci, tap:tap + 1], acc[:], ALU.mult, ALU.add),
                         reads=[pre[i], convw, acc], writes=[acc])
                if i == 2:
                    k.op("act", lambda e: e.activation(post[2][:, o:o + 512], acc[:], AF.Silu), reads=[acc], writes=[post[2]])
                    continue
                sl, sq, rn = sl_r.next(), sq_r.next(), rn_r.next()
                k.op("act", lambda e: e.activation(sl[:], acc[:], AF.Silu), reads=[acc], writes=[sl])
                k.op("act", lambda e: e.activation(sq[:], sl[:], AF.Square), reads=[sl], writes=[sq])
                k.op("pe", lambda e: e.matmul(pBig[:], onesb[:], sq[:], start=True, stop=True), reads=[onesb, sq], writes=[pBig])
                k.op("dve", lambda e: e.tensor_scalar(rn[:], pBig[:], 1e-6, None, ALU.add), reads=[pBig], writes=[rn])
                rsqrt_inplace(k, rn, rn[:])
                sc_ = (128.0 ** -0.5) if i == 0 else 1.0
                k.op("dve", lambda e: e.scalar_tensor_tensor(post[i][:, o:o + 512], sl[:], sc_, rn[:], ALU.mult, ALU.mult), reads=[sl, rn], writes=[post[i]])
        qT, kT, vT = post
        if DBG.get("p2stop") == 2:
            k.barrier(); P.close(); return
        k.op("pool", lambda e: e.memset(Sst[:], 0.0), writes=[Sst])
        k.op("pool", lambda e: e.memset(Sbf[:], 0.0), writes=[Sbf])
        for c in range(DBG.get("nch", NT)):
            c0 = c * 128
            cs = slice(c0, c0 + 128)
            g_col, b_col, nb_col = G[:, c, h:h + 1], BETA[:, c, h:h + 1], NBETA[:, c, h:h + 1]
            Gm, E = Gm_r.next(), E_r.next()
            k.op("dve", lambda e: e.tensor_scalar(Gm[:], UTI, g_col, None, ALU.mult), reads=[CM, G], writes=[Gm])
            k.op("pe", lambda e: e.matmul(pA[:, 0:128], LTS, Gm[:], start=True, stop=True), reads=[CM, Gm], writes=[pA])
            k.op("pe", lambda e: e.matmul(pA[:, 128:256], ONES, Gm[:], start=True, stop=True), reads=[CM, Gm], writes=[pA])
            k.op("act", lambda e: e.activation(E[:], pA[:], AF.Exp), reads=[pA], writes=[E])
            if DBG.get("cstop") == 1:
                break
            DTc, DTs, Y = DTc_r.next(), DTs_r.next(), Y_r.next()
            k.op("pool", lambda e: e.tensor_tensor(DTc[:], E[:, 0:128], UTI, ALU.mult), reads=[E, CM], writes=[DTc])
            k.op("pool", lambda e: e.tensor_tensor(DTs[:], E[:, 0:128], UTS, ALU.mult), reads=[E, CM], writes=[DTs])
            col = col_r.next()
            k.op("dve", lambda e: e.scalar_tensor_tensor(junk[:], E[:, 128:256], 1.0, IDENT, ALU.mult, ALU.mult, accum_out=col[:, 0:1]),
                 reads=[E, CM], writes=[junk, col])
            if DBG.get("cstop") == 2:
                break
            k.op("pe", lambda e: e.transpose(pT[:, 0:128], kT[:, cs], identb[:]), reads=[kT, identb], writes=[pT])
            k.op("pe", lambda e: e.transpose(pT[:, 128:256], vT[:, cs], identb[:]), reads=[vT, identb], writes=[pT])
            kg, kt, vt = kg_r.next(), kt_r.next(), vt_r.next()
            k.op("act", lambda e: e.activation(kg[:], pT[:, 0:128], AF.Copy, scale=col[:, 0:1]), reads=[pT, col], writes=[kg])
            k.op("act", lambda e: e.activation(kt[:], pT[:, 0:128], AF.Copy, scale=E[:, 127:128]), reads=[pT, E], writes=[kt])
            k.op("dve", lambda e: e.tensor_copy(vt[:], pT[:, 128:256]), reads=[pT], writes=[vt])
            if DBG.get("cstop") == 3:
                break
            k.op("pe", lambda e: e.matmul(pK[:, 0:128], kT[:, cs], kT[:, cs], start=True, stop=True), reads=[kT], writes=[pK])
            k.op("pe", lambda e: e.matmul(pK[:, 128:256], kT[:, cs], qT[:, cs], start=True, stop=True), reads=[kT, qT], writes=[pK])
            P2 = P2_r.next()
            k.op("dve", lambda e: e.scalar_tensor_tensor(Y[:], pK[:, 0:128], b_col, DTs[:], ALU.mult, ALU.mult), reads=[pK, BETA, DTs], writes=[Y])
            k.op("dve", lambda e: e.tensor_tensor(P2[:], pK[:, 128:256], DTc[:], ALU.mult), reads=[pK, DTc], writes=[P2])
            if DBG.get("cstop") == 4:
                break
            pI = pI_r.next()
            Z = Z_r.next()
            k.op("pe", lambda e: e.matmul(pI[:], Y[:], IDENT, start=True, stop=True), reads=[Y, CM], writes=[pI])
            k.op("dve", lambda e: e.tensor_copy(Z[:], pI[:]), reads=[pI], writes=[Z])
            W = Y
            Pm = Pm_r.next()
            k.op("dve", lambda e: e.tensor_tensor(Pm[:], IDENT, Y[:], ALU.subtract), reads=[CM, Y], writes=[Pm])
            for lev in range(1, 7):
                if lev > DBG.get('nlev', 6):
                    break
                Wn = Zn = None
                if lev < 6:
                    pW = pI_r.next()
                    Wn = W_r.next()
                    k.op("pe", lambda e: e.matmul(pW[:], Z[:], W[:], start=True, stop=True), reads=[Z, W], writes=[pW])
                    k.op("dve", lambda e: e.tensor_copy(Wn[:], pW[:]), reads=[pW], writes=[Wn])
                pZ = pI_r.next()
                IZ = IZ_r.next()
                k.op("pe", lambda e: e.matmul(pZ[:], W[:], Z[:], start=True, stop=True), reads=[Z, W], writes=[pZ])
                k.op("dve", lambda e: e.tensor_tensor(IZ[:], pZ[:], IDENT, ALU.add), reads=[pZ, CM], writes=[IZ])
                if lev < 6:
                    Zn = Z_r.next()
                    k.op("dve", lambda e: e.tensor_copy(Zn[:], pZ[:]), reads=[pZ], writes=[Zn])
                pP = pI_r.next()
                k.op("pe", lambda e: e.matmul(pP[:], IZ[:], Pm[:], start=True, stop=True), reads=[IZ, Pm], writes=[pP])
                if lev < 6:
                    Pn = Pm_r.next()
                    k.op("dve", lambda e: e.tensor_copy(Pn[:], pP[:]), reads=[pP], writes=[Pn])
                    Pm = Pn
                    W, Z = Wn, Zn
                else:
                    T2 = T2_r.next()
                    k.op("dve", lambda e: e.tensor_copy(T2[:], pP[:]), reads=[pP], writes=[T2])
            if DBG.get("cstop") == 5:
                break
            u, wT, qd, vn = u_r.next(), wT_r.next(), qd_r.next(), vn_r.next()
            pU = pI_r.next()
            k.op("pe", lambda e: e.matmul(pU[:], T2[:], vt[:], start=True, stop=True), reads=[T2, vt], writes=[pU])
            k.op("act", lambda e: e.activation(u[:], pU[:], AF.Copy, scale=b_col), reads=[pU, BETA], writes=[u])
            pWt = pI_r.next()
            k.op("pe", lambda e: e.matmul(pWt[:], kg[:], T2[:], start=True, stop=True), reads=[T2, kg], writes=[pWt])
            k.op("act", lambda e: e.activation(wT[:], pWt[:], AF.Copy), reads=[pWt], writes=[wT])
            k.op("pool", lambda e: e.tensor_tensor(qd[:], qT[:, cs], E[:, 128:256], ALU.mult), reads=[qT, E], writes=[qd])
            if DBG.get("cstop") == 6:
                break
            k.op("pe", lambda e: e.matmul(pS1[:], wT[:], Sbf[:], start=True, stop=True), reads=[wT, Sbf], writes=[pS1])
            k.op("dve", lambda e: e.scalar_tensor_tensor(vn[:], pS1[:], nb_col, u[:], ALU.mult, ALU.add), reads=[pS1, NBETA, u], writes=[vn])
            k.op("pe", lambda e: e.matmul(pBig[:, 0:128], qd[:], Sbf[:], start=True, stop=False), reads=[qd, Sbf], writes=[pBig])
            k.op("pe", lambda e: e.matmul(pBig[:, 0:128], P2[:], vn[:], start=False, stop=True), reads=[P2, vn], writes=[pBig])
            k.op("pe", lambda e: e.matmul(pS2[:], kt[:], vn[:], start=True, stop=True), reads=[kt, vn], writes=[pS2])
            k.op("dve", lambda e: e.scalar_tensor_tensor(Sst[:], Sst[:], E[:, 255:256], pS2[:], ALU.mult, ALU.add), reads=[Sst, E, pS2], writes=[Sst])
            k.op("act", lambda e: e.activation(Sbf[:], Sst[:], AF.Copy), reads=[Sst], writes=[Sbf])
            if DBG.get("cstop") == 7:
                break
            k.op("act", lambda e: e.activation(junk[:], pBig[:, 0:128], AF.Square, accum_out=col[:, 1:2]), reads=[pBig], writes=[junk, col])
            k.op("dve", lambda e: e.tensor_scalar(col[:, 2:3], col[:, 1:2], 1.0 / 128, 1e-6, ALU.mult, ALU.add), reads=[col], writes=[col])
            rsqrt_inplace(k, col, col[:, 2:3])
            gz, ych = gz_r.next(), y_r.next()
            k.op("pool", lambda e: e.tensor_tensor(gz[:], ZS[:, c, :], normg[:], ALU.mult), reads=[ZS, normg], writes=[gz])
            k.op("dve", lambda e: e.scalar_tensor_tensor(ych[:], pBig[:, 0:128], col[:, 2:3], gz[:], ALU.mult, ALU.mult), reads=[pBig, col, gz], writes=[ych])
            k.dma("sync", T["Y"][c0:c0 + 128, h * 128:(h + 1) * 128], ych[:], reads=[ych])
    k.new_epoch()
    P.close()


def phase3(k, nc, T, L):
    P = Pool(nc)
    CM, identb, onesb = load_consts(k, P, T)
    IDENT, UTI, LTS, UTS, ONES, NEGM, SEL = (CM[:, i, :] for i in range(7))
    gb = P.sb([128, 8], F32)
    k.dma("sync", gb[:], T["mlstm_gate_bias"][L].partition_broadcast(128), writes=[gb])
    normg = P.sb([128, 512], F32)
    k.dma("sync", normg[:], T["mlstm_norm"][L].partition_broadcast(128), writes=[normg])
    TS = load_ts(k, P, T)
    IL = P.sb([128, NT, 4], F32, "IL")
    FL = P.sb([128, NT, 4], F32, "FL")
    for h in range(4):
        k.op("dve", lambda e: e.tensor_scalar(IL[:, :, h], TS[:, :, 8 + h], gb[:, h:h + 1], None, ALU.add), reads=[TS, gb], writes=[IL])
        k.op("dve", lambda e: e.tensor_scalar(FL[:, :, h], TS[:, :, 12 + h], gb[:, 4 + h:5 + h], None, ALU.add), reads=[TS, gb], writes=[FL])
    for B_ in (IL, FL):
        k.op("act", lambda e: e.activation(B_[:], B_[:], AF.Tanh, scale=1.0 / 15.0), reads=[B_], writes=[B_])
        k.op("dve", lambda e: e.tensor_scalar(B_[:], B_[:], 15.0, None, ALU.mult), reads=[B_], writes=[B_])
    k.op("act", lambda e: e.activation(FL[:], FL[:], AF.Exp, scale=-1.0), reads=[FL], writes=[FL])
    k.op("dve", lambda e: e.tensor_scalar(FL[:], FL[:], 1.0, None, ALU.add), reads=[FL], writes=[FL])
    k.op("act", lambda e: e.activation(FL[:], FL[:], AF.Ln), reads=[FL], writes=[FL])
    k.op("dve", lambda e: e.tensor_scalar(FL[:], FL[:], -1.0, None, ALU.mult), reads=[FL], writes=[FL])

    qT = P.sb([128, S], BF16, "qT")
    kT = P.sb([128, S], BF16, "kT")
    V1 = P.sb([128, NT, 130], BF16, "V1")
    OS = P.sb([128, NT, 128], BF16, "OS")
    k.op("pool", lambda e: e.memset(V1[:], 1.0), writes=[V1])
    pA = P.ps([128, 256], F32, "pA")
    pB = P.ps([128, 128], F32, "pB")
    pK = P.ps([128, 128], F32, "pK")
    pT = P.ps([128, 256], BF16, "pT")
    pNA = P.ps([128, 130], F32, "pNA")
    pNB = P.ps([128, 130], F32, "pNB")
    pC = P.ps([128, 130], F32, "pC")

    def f32t(n, name, w=128):
        return Rot([P.sb([128, w], F32, name) for _ in range(n)])

    def bft(n, name):
        return Rot([P.sb([128, 128], BF16, name) for _ in range(n)])

    Fm_r, Di_r, ld_r, E_r = f32t(2, "Fm"), f32t(2, "Di"), f32t(2, "ld"), f32t(2, "E")
    NB_r, comb_r, hh_r = f32t(2, "NB", 130), f32t(2, "comb", 130), f32t(2, "hh")
    Sb_r, ST_r, kk_r, go_r, y_r = (bft(2, n) for n in ("Sb", "ST", "kk", "go", "ych"))
    col_r = Rot([P.sb([128, 16], F32, "col") for _ in range(3)])
    junk = P.sb([128, 128], F32, "junk")
    Cn = P.sb([128, 130], F32, "Cn")
    Cnb = P.sb([128, 130], BF16, "Cnb")
    mprev = P.sb([128, 1], F32, "mprev")
    Thv = T["Th"].rearrange("(c p) d -> p c d", p=128)
    for h in range(DBG.get("nh", 4)):
        for q in range(4):
            cs = slice(q * 2048, (q + 1) * 2048)
            k.dma("sync", qT[:, cs], T["F"][R_MQ + h * 128:R_MQ + (h + 1) * 128, cs], writes=[qT])
            k.dma("sync", kT[:, cs], T["F"][R_MK + h * 128:R_MK + (h + 1) * 128, cs], writes=[kT])
            k.dma("sync", V1[:, q * 16:(q + 1) * 16, 0:128], Thv[:, q * 16:(q + 1) * 16, C_MV + h * 128:C_MV + (h + 1) * 128], writes=[V1])
            k.dma("sync", OS[:, q * 16:(q + 1) * 16, :], Thv[:, q * 16:(q + 1) * 16, C_MO + h * 128:C_MO + (h + 1) * 128], writes=[OS])
        for q in range(4):
            cs = slice(q * 2048, (q + 1) * 2048)
            k.op("act", lambda e: e.activation(kT[:, cs], kT[:, cs], AF.Copy, scale=128.0 ** -0.5), reads=[kT], writes=[kT])
            k.op("act", lambda e: e.activation(OS[:, q * 16:(q + 1) * 16, :], OS[:, q * 16:(q + 1) * 16, :], AF.Sigmoid), reads=[OS], writes=[OS])
        k.op("pool", lambda e: e.memset(Cn[:], 0.0), writes=[Cn])
        k.op("pool", lambda e: e.memset(Cnb[:], 0.0), writes=[Cnb])
        k.op("pool", lambda e: e.memset(mprev[:], 0.0), writes=[mprev])
        for c in range(DBG.get("nch", NT)):
            c0 = c * 128
            cs = slice(c0, c0 + 128)
            f_col, i_col = FL[:, c, h:h + 1], IL[:, c, h:h + 1]
            Fm, Di, ld, E = Fm_r.next(), Di_r.next(), ld_r.next(), E_r.next()
            col = col_r.next()
            k.op("dve", lambda e: e.tensor_scalar(Fm[:], UTI, f_col, None, ALU.mult), reads=[CM, FL], writes=[Fm])
            k.op("dve", lambda e: e.tensor_scalar(Di[:], IDENT, i_col, None, ALU.mult), reads=[CM, IL], writes=[Di])
            k.op("pe", lambda e: e.matmul(pA[:, 0:128], Fm[:], LTS, start=True, stop=False), reads=[Fm, CM], writes=[pA])
            k.op("pe", lambda e: e.matmul(pA[:, 0:128], ONES, Di[:], start=False, stop=True), reads=[Di, CM], writes=[pA])
            k.op("pe", lambda e: e.matmul(pA[:, 128:256], ONES, Fm[:], start=True, stop=True), reads=[Fm, CM], writes=[pA])
            k.op("dve", lambda e: e.tensor_tensor(ld[:], pA[:, 0:128], NEGM, ALU.add), reads=[pA, CM], writes=[ld])
            k.op("dve", lambda e: e.scalar_tensor_tensor(junk[:], pA[:, 128:256], 1.0, IDENT, ALU.mult, ALU.mult, accum_out=col[:, 0:1]), reads=[pA, CM], writes=[junk, col])
            k.op("act", lambda e: e.activation(col[:, 1:2], pA[:, 255:256], AF.Copy), reads=[pA], writes=[col])
            k.op("dve", lambda e: e.reduce_max(col[:, 2:3], ld[:], AX.X), reads=[ld], writes=[col])
            k.op("pe", lambda e: e.matmul(pB[:], SEL, ld[:], start=True, stop=True), reads=[ld, CM], writes=[pB])
            k.op("dve", lambda e: e.reduce_max(col[:, 3:4], pB[:], AX.X), reads=[pB], writes=[col])
            k.op("dve", lambda e: e.scalar_tensor_tensor(col[:, 4:5], col[:, 0:1], mprev[:, 0:1], col[:, 2:3], ALU.add, ALU.max), reads=[col, mprev], writes=[col])
            k.op("dve", lambda e: e.tensor_scalar(col[:, 5:6], col[:, 4:5], -1.0, None, ALU.mult), reads=[col], writes=[col])
            k.op("dve", lambda e: e.scalar_tensor_tensor(col[:, 6:7], col[:, 0:1], mprev[:, 0:1], col[:, 5:6], ALU.add, ALU.add), reads=[col, mprev], writes=[col])
            k.op("act", lambda e: e.activation(col[:, 6:7], col[:, 6:7], AF.Exp), reads=[col], writes=[col])
            k.op("act", lambda e: e.activation(E[:], ld[:], AF.Exp, bias=col[:, 5:6]), reads=[ld, col], writes=[E])
            k.op("pe", lambda e: e.matmul(pK[:], qT[:, cs], kT[:, cs], start=True, stop=True), reads=[qT, kT], writes=[pK])
            Sb, ST = Sb_r.next(), ST_r.next()
            k.op("dve", lambda e: e.tensor_tensor(Sb[:], pK[:], E[:], ALU.mult), reads=[pK, E], writes=[Sb])
            k.op("pe", lambda e: e.transpose(pT[:, 0:128], Sb[:], identb[:]), reads=[Sb, identb], writes=[pT])
            k.op("act", lambda e: e.activation(ST[:], pT[:, 0:128], AF.Copy), reads=[pT], writes=[ST])
            k.op("pe", lambda e: e.matmul(pNA[:, 0:130], qT[:, cs], Cnb[:, 0:130], start=True, stop=True), reads=[qT, Cnb], writes=[pNA])
            k.op("pe", lambda e: e.matmul(pNB[:, 0:130], ST[:], V1[:, c, 0:130], start=True, stop=True), reads=[ST, V1], writes=[pNB])
            NB, comb, hh = NB_r.next(), comb_r.next(), hh_r.next()
            k.op("act", lambda e: e.activation(NB[:, 0:130], pNB[:, 0:130], AF.Copy), reads=[pNB], writes=[NB])
            k.op("dve", lambda e: e.scalar_tensor_tensor(comb[:, 0:130], pNA[:, 0:130], col[:, 6:7], NB[:, 0:130], ALU.mult, ALU.add), reads=[pNA, col, NB], writes=[comb])
            k.op("act", lambda e: e.activation(col[:, 7:8], comb[:, 128:129], AF.Abs), reads=[comb], writes=[col])
            k.op("act", lambda e: e.activation(col[:, 8:9], col[:, 5:6], AF.Exp), reads=[col], writes=[col])
            k.op("dve", lambda e: e.tensor_tensor(col[:, 9:10], col[:, 7:8], col[:, 8:9], ALU.max), reads=[col], writes=[col])
            k.op("dve", lambda e: e.reciprocal(col[:, 9:10], col[:, 9:10]), reads=[col], writes=[col])
            k.op("dve", lambda e: e.tensor_scalar(hh[:], comb[:, 0:128], col[:, 9:10], None, ALU.mult), reads=[comb, col], writes=[hh])
            k.op("act", lambda e: e.activation(junk[:], hh[:], AF.Square, accum_out=col[:, 10:11]), reads=[hh], writes=[junk, col])
            k.op("dve", lambda e: e.tensor_scalar(col[:, 11:12], col[:, 10:11], 1.0 / 128, 1e-6, ALU.mult, ALU.add), reads=[col], writes=[col])
            rsqrt_inplace(k, col, col[:, 11:12])
            go, ych = go_r.next(), y_r.next()
            k.op("pool", lambda e: e.tensor_tensor(go[:], OS[:, c, :], normg[:, h * 128:(h + 1) * 128], ALU.mult), reads=[OS, normg], writes=[go])
            k.op("dve", lambda e: e.scalar_tensor_tensor(ych[:], hh[:], col[:, 11:12], go[:], ALU.mult, ALU.mult), reads=[hh, col, go], writes=[ych])
            k.dma("sync", T["Y"][c0:c0 + 128, 512 + h * 128:512 + (h + 1) * 128], ych[:], reads=[ych])
            k.op("dve", lambda e: e.scalar_tensor_tensor(col[:, 12:13], col[:, 1:2], mprev[:, 0:1], col[:, 3:4], ALU.add, ALU.max), reads=[col, mprev], writes=[col])
            k.op("dve", lambda e: e.tensor_scalar(col[:, 13:14], col[:, 12:13], -1.0, None, ALU.mult), reads=[col], writes=[col])
            k.op("dve", lambda e: e.scalar_tensor_tensor(col[:, 14:15], col[:, 1:2], mprev[:, 0:1], col[:, 13:14], ALU.add, ALU.add), reads=[col, mprev], writes=[col])
            k.op("act", lambda e: e.activation(col[:, 14:15], col[:, 14:15], AF.Exp), reads=[col], writes=[col])
            k.op("dve", lambda e: e.scalar_tensor_tensor(col[:, 15:16], col[:, 0:1], -1.0, col[:, 1:2], ALU.mult, ALU.add), reads=[col], writes=[col])
            k.op("dve", lambda e: e.tensor_tensor(col[:, 15:16], col[:, 15:16], i_col, ALU.add), reads=[col, IL], writes=[col])
            k.op("act", lambda e: e.activation(col[:, 15:16], col[:, 15:16], AF.Exp, bias=col[:, 13:14]), reads=[col], writes=[col])
            kk = kk_r.next()
            k.op("pe", lambda e: e.transpose(pT[:, 128:256], kT[:, cs], identb[:]), reads=[kT, identb], writes=[pT])
            k.op("act", lambda e: e.activation(kk[:], pT[:, 128:256], AF.Copy, scale=col[:, 15:16]), reads=[pT, col], writes=[kk])
            k.op("pe", lambda e: e.matmul(pC[:, 0:130], kk[:], V1[:, c, 0:130], start=True, stop=True), reads=[kk, V1], writes=[pC])
            k.op("dve", lambda e: e.scalar_tensor_tensor(Cn[:, 0:130], Cn[:, 0:130], col[:, 14:15], pC[:, 0:130], ALU.mult, ALU.add), reads=[Cn, col, pC], writes=[Cn])
            k.op("act", lambda e: e.activation(Cnb[:, 0:130], Cn[:, 0:130], AF.Copy), reads=[Cn], writes=[Cnb])
            k.op("dve", lambda e: e.tensor_copy(mprev[:], col[:, 12:13]), reads=[col], writes=[mprev])
    k.new_epoch()
    P.close()


def phase4(k, nc, T, L):
    P = Pool(nc)
    SC = 192.0 ** -0.5
    onesb = P.sb([128, 128], BF16, "onesb")
    k.dma("gp", onesb[:], T["c_mats"][:, 4, :], writes=[onesb])
    wuq = P.sb([128, 3, 1024], BF16, "wuq")
    wukv = P.sb([128, 2, 1024], BF16, "wukv")
    for c in range(3):
        k.dma("gp", wuq[:, c, :], T["wuq"][L, c * 128:(c + 1) * 128, :], writes=[wuq])
    for c in range(2):
        k.dma("gp", wukv[:, c, :], T["mla_w_ukv"][L, c * 128:(c + 1) * 128, :], writes=[wukv])
    qg = P.sb([128, 3], F32)
    kvg = P.sb([128, 2], F32)
    rc = P.sb([64, 3], F32)
    k.dma("sync", qg[:], T["qnormT"][L], writes=[qg])
    k.dma("sync", kvg[:], T["kvnormT"][L], writes=[kvg])
    k.dma("sync", rc[:], T["c_rope"][:, :], writes=[rc])
    pS_r = Rot([P.ps([128, 512], F32, "pS") for _ in range(2)])
    pO = [P.ps([128, 512], F32, "pO") for _ in range(4)]
    with contextlib.ExitStack() as stA:
        PA = Pool(nc)
        stA.callback(PA.close)
        cq_r = Rot([PA.sb([128, 3, 512], BF16, "cq") for _ in range(2)])
        ckv_r = Rot([PA.sb([128, 2, 512], BF16, "ckv") for _ in range(2)])
        cqn_r = Rot([PA.sb([128, 3, 512], BF16, "cqn") for _ in range(2)])
        ckvn_r = Rot([PA.sb([128, 2, 512], BF16, "ckvn") for _ in range(2)])
        kr_r = Rot([PA.sb([64, 2, 512], BF16, "kr") for _ in range(2)])
        sq_r = Rot([PA.sb([128, 512], BF16, "sq") for _ in range(3)])
        rn_r = Rot([PA.sb([128, 512], F32, "rn") for _ in range(2)])
        posi_r = Rot([PA.sb([64, 512], I32, "posi") for _ in range(2)])
        ang_r = Rot([PA.sb([64, 512], F32, "ang") for _ in range(2)])
        cs_r = Rot([PA.sb([64, 2, 512], F32, "cs") for _ in range(2)])
        t_r = Rot([PA.sb([64, 512], F32, "t64") for _ in range(4)])
        st128_r = Rot([PA.sb([128, 512], BF16, "st128") for _ in range(4)])
        st64_r = Rot([PA.sb([64, 512], BF16, "st64") for _ in range(4)])
        stv_r = Rot([PA.sb([128, 512], BF16, "stv") for _ in range(2)])
        Fv = T["F"]
        for ti in range(DBG.get('nqi', S // 512)):
            ts_ = slice(ti * 512, (ti + 1) * 512)
            cq, ckv, cqn, ckvn, kr = cq_r.next(), ckv_r.next(), cqn_r.next(), ckvn_r.next(), kr_r.next()
            for c in range(3):
                k.dma("sync", cq[:, c, :], Fv[R_CQ + c * 128:R_CQ + (c + 1) * 128, ts_], writes=[cq])
            for c in range(2):
                k.dma("sync", ckv[:, c, :], Fv[R_CKV + c * 128:R_CKV + (c + 1) * 128, ts_], writes=[ckv])
            k.dma("sync", kr[:, 0, :], Fv[R_KR:R_KR + 64, ts_], writes=[kr])
            k.dma("sync", kr[:, 1, :], Fv[R_KRS:R_KRS + 64, ts_], writes=[kr])
            for (src, dst, nch, gcol, dim) in ((cq, cqn, 3, qg, 384.0), (ckv, ckvn, 2, kvg, 256.0)):
                pp = pS_r.next()
                for c in range(nch):
                    sq = sq_r.next()
                    k.op("act", lambda e: e.activation(sq[:], src[:, c, :], AF.Square), reads=[src], writes=[sq])
                    k.op("pe", lambda e: e.matmul(pp[:], onesb[:], sq[:], start=(c == 0), stop=(c == nch - 1)), reads=[onesb, sq], writes=[pp])
                rn = rn_r.next()
                k.op("dve", lambda e: e.tensor_scalar(rn[:], pp[:], 1.0 / dim, 1e-6, ALU.mult, ALU.add), reads=[pp], writes=[rn])
                rsqrt_inplace(k, rn, rn[:])
                for c in range(nch):
                    k.op("dve", lambda e: e.scalar_tensor_tensor(dst[:, c, :], src[:, c, :], gcol[:, c:c + 1], rn[:], ALU.mult, ALU.mult),
                         reads=[src, gcol, rn], writes=[dst])
            posi, ang, cs = posi_r.next(), ang_r.next(), cs_r.next()
            k.dma("sync", posi[:], T["positions"][ts_].partition_broadcast(64), writes=[posi])
            k.op("dve", lambda e: e.tensor_copy(ang[:], posi[:]), reads=[posi], writes=[ang])
            k.op("dve", lambda e: e.tensor_scalar(ang[:], ang[:], rc[:, 0:1], None, ALU.mult), reads=[ang, rc], writes=[ang])
            for j in range(2):
                uu, uf = t_r.next(), t_r.next()
                k.op("dve", lambda e: e.tensor_scalar(uu[:], ang[:], rc[:, 1 + j:2 + j], 1.0 / (2.0 * math.pi), ALU.add, ALU.mult), reads=[ang, rc], writes=[uu])
                k.op("dve", lambda e: e.tensor_copy(posi[:], uu[:]), reads=[uu], writes=[posi])
                k.op("dve", lambda e: e.tensor_copy(uf[:], posi[:]), reads=[posi], writes=[uf])
                k.op("dve", lambda e: e.tensor_tensor(uu[:], uu[:], uf[:], ALU.subtract), reads=[uu, uf], writes=[uu])
                k.op("act", lambda e: e.activation(cs[:, j, :], uu[:], AF.Sin, scale=2.0 * math.pi), reads=[uu], writes=[cs])
            t1, t2 = t_r.next(), t_r.next()
            k.op("dve", lambda e: e.tensor_tensor(t1[:], kr[:, 0, :], cs[:, 0, :], ALU.mult), reads=[kr, cs], writes=[t1])
            k.op("dve", lambda e: e.tensor_tensor(t2[:], kr[:, 1, :], cs[:, 1, :], ALU.mult), reads=[kr, cs], writes=[t2])
            st = st64_r.next()
            k.op("dve", lambda e: e.tensor_tensor(st[:], t1[:], t2[:], ALU.add), reads=[t1, t2], writes=[st])
            k.dma("sync", T["MKR"][:, ts_], st[:], reads=[st])
            for h in range(4):
                w0 = h * 256
                pp = pS_r.next()
                for c in range(3):
                    k.op("pe", lambda e: e.matmul(pp[:], wuq[:, c, w0:w0 + 128], cqn[:, c, :], start=(c == 0), stop=(c == 2)), reads=[wuq, cqn], writes=[pp])
                st = st128_r.next()
                k.op("act", lambda e: e.activation(st[:], pp[:], AF.Copy, scale=SC), reads=[pp], writes=[st])
                k.dma("sync", T["MQ"][h, 0:128, ts_], st[:], reads=[st])
                pp = pS_r.next()
                for c in range(3):
                    k.op("pe", lambda e: e.matmul(pp[0:64, :], wuq[:, c, w0 + 128:w0 + 192], cqn[:, c, :], start=(c == 0), stop=(c == 2)), reads=[wuq, cqn], writes=[pp])
                pp2 = pS_r.next()
                for c in range(3):
                    k.op("pe", lambda e: e.matmul(pp2[0:64, :], wuq[:, c, w0 + 192:w0 + 256], cqn[:, c, :], start=(c == 0), stop=(c == 2)), reads=[wuq, cqn], writes=[pp2])
                t1, t2 = t_r.next(), t_r.next()
                k.op("dve", lambda e: e.tensor_tensor(t1[:], pp[0:64, :], cs[:, 0, :], ALU.mult), reads=[pp, cs], writes=[t1])
                k.op("dve", lambda e: e.tensor_tensor(t2[:], pp2[0:64, :], cs[:, 1, :], ALU.mult), reads=[pp2, cs], writes=[t2])
                st = st64_r.next()
                k.op("dve", lambda e: e.scalar_tensor_tensor(st[:], t1[:], SC, t2[:], ALU.mult, ALU.add), reads=[t1, t2], writes=[st])
                k.op("dve", lambda e: e.scalar_tensor_tensor(st[:], t2[:], SC - 1.0, st[:], ALU.mult, ALU.add), reads=[t2, st], writes=[st])
                k.dma("sync", T["MQ"][h, 128:192, ts_], st[:], reads=[st])
                pp = pS_r.next()
                for c in range(2):
                    k.op("pe", lambda e: e.matmul(pp[:], wukv[:, c, w0:w0 + 128], ckvn[:, c, :], start=(c == 0), stop=(c == 1)), reads=[wukv, ckvn], writes=[pp])
                st = st128_r.next()
                k.op("act", lambda e: e.activation(st[:], pp[:], AF.Copy), reads=[pp], writes=[st])
                k.dma("sync", T["MK"][h, :, ts_], st[:], reads=[st])
                pp = pS_r.next()
                for s_ in range(4):
                    for c in range(2):
                        k.op("pe", lambda e: e.matmul(pp[:, s_ * 128:(s_ + 1) * 128], ckvn[:, c, s_ * 128:(s_ + 1) * 128], wukv[:, c, w0 + 128:w0 + 256], start=(c == 0), stop=(c == 1)),
                             reads=[wukv, ckvn], writes=[pp])
                stv = stv_r.next()
                k.op("act", lambda e: e.activation(stv[:], pp[:], AF.Copy), reads=[pp], writes=[stv])
                k.dma("sync", T["MV"][h].rearrange("(c p) d -> p c d", p=128)[:, ti * 4:(ti + 1) * 4, :], stv[:, :].rearrange("p (c d) -> p c d", c=4), reads=[stv])
        k.barrier()
    QN = P.sb([128, S], BF16, "QN")
    QR = P.sb([64, S], BF16, "QR")
    KN = P.sb([128, S], BF16, "KN")
    KR = P.sb([64, S], BF16, "KR")
    V1 = P.sb([128, NT, 130], BF16, "V1")
    MASK = P.sb([128, 4, 512], BF16, "MASK")
    k.dma("gp", MASK[:], T["c_amask"][:, :, :], writes=[MASK])
    k.op("pool", lambda e: e.memset(V1[:], 1.0), writes=[V1])
    for q in range(4):
        cs_ = slice(q * 2048, (q + 1) * 2048)
        k.dma("sync", KR[:, cs_], T["MKR"][:, cs_], writes=[KR])
    PT_r = Rot([P.sb([128, 512], BF16, "PT") for _ in range(3)])
    yst_r = Rot([P.sb([128, 128], BF16, "yst") for _ in range(4)])
    col_r = Rot([P.sb([128, 1], F32, "rc") for _ in range(4)])
    for h in range(DBG.get('nh', 4)):
        for q in range(4):
            cs_ = slice(q * 2048, (q + 1) * 2048)
            k.dma("sync", QN[:, cs_], T["MQ"][h, 0:128, cs_], writes=[QN])
            k.dma("sync", QR[:, cs_], T["MQ"][h, 128:192, cs_], writes=[QR])
            k.dma("sync", KN[:, cs_], T["MK"][h, :, cs_], writes=[KN])
            k.dma("sync", V1[:, q * 16:(q + 1) * 16, 0:128], T["MV"][h].rearrange("(c p) d -> p c d", p=128)[:, q * 16:(q + 1) * 16, :], writes=[V1])
        for qi in range(DBG.get('nqi', S // 512)):
            qs = slice(qi * 512, (qi + 1) * 512)
            nkb = 4 * qi + 4
            def emit_s(kb_):
                ks_ = slice(kb_ * 128, (kb_ + 1) * 128)
                pS_ = pS_r.next()
                k.op("pe", lambda e: e.matmul(pS_[:], KN[:, ks_], QN[:, qs], start=True, stop=False), reads=[KN, QN], writes=[pS_])
                k.op("pe", lambda e: e.matmul(pS_[:], KR[:, ks_], QR[:, qs], start=False, stop=True), reads=[KR, QR], writes=[pS_])
                return pS_
            pS_next = emit_s(0)
            for kb in range(nkb):
                pS = pS_next
                if kb + 1 < nkb:
                    pS_next = emit_s(kb + 1)
                PT = PT_r.next()
                k.op("act", lambda e: e.activation(PT[:], pS[:], AF.Exp), reads=[pS], writes=[PT])
                j = kb - 4 * qi
                if j >= 0:
                    k.op("pool", lambda e: e.tensor_tensor(PT[:], PT[:], MASK[:, j, :], ALU.mult), reads=[PT, MASK], writes=[PT])
                for s_ in range(4):
                    if j > s_:
                        continue
                    k.op("pe", lambda e: e.matmul(pO[s_][:, 0:130], PT[:, s_ * 128:(s_ + 1) * 128], V1[:, kb, 0:130], start=(kb == 0), stop=(kb == 4 * qi + s_)),
                         reads=[PT, V1], writes=[pO[s_]])
            for s_ in range(4):
                col, yst = col_r.next(), yst_r.next()
                k.op("dve", lambda e: e.reciprocal(col[:], pO[s_][:, 128:129]), reads=[pO[s_]], writes=[col])
                k.op("act", lambda e: e.activation(yst[:], pO[s_][:, 0:128], AF.Copy, scale=col[:]), reads=[pO[s_], col], writes=[yst])
                r0 = qi * 512 + s_ * 128
                k.dma("sync", T["Y"][r0:r0 + 128, 1024 + h * 128:1024 + (h + 1) * 128], yst[:], reads=[yst])
    k.new_epoch()
    P.close()


def phase0(k, nc, T, L):
    P = Pool(nc)
    a_r = Rot([P.sb([128, 8, 512], BF16, "cva") for _ in range(3)])
    b_r = Rot([P.sb([128, 4, 1024], BF16, "cvb") for _ in range(2)])
    for e_ in range(16):
        for nm, dst in (("moe_w1", "EW1"), ("moe_w3", "EW3")):
            a = a_r.next()
            k.dma("gp", a[:], T[nm][L, e_].rearrange("(k p) n -> p k n", p=128), writes=[a])
            k.dma("sync", T[dst][e_].rearrange("(k p) n -> p k n", p=128), a[:], reads=[a])
        b = b_r.next()
        k.dma("gp", b[:], T["moe_w2"][L, e_].rearrange("(k p) n -> p k n", p=128), writes=[b])
        k.dma("sync", T["EW2"][e_].rearrange("(k p) n -> p k n", p=128), b[:], reads=[b])
    k.new_epoch()
    P.close()


def phase5(k, nc, T, L, last):
    P = Pool(nc)
    identb = P.sb([128, 128], BF16, "identb")
    k.dma("gp", identb[:], T["c_ident"][:, :], writes=[identb])
    WBR = P.sb([128, 12, 1024], BF16, "WBR")
    WOUT = P.sb([128, 8, 1024], BF16, "WOUT")
    RW = P.sb([128, 8, 16], BF16, "RW")
    for b in range(3):
        k.dma("gp", WBR[:, b * 4:(b + 1) * 4, :], T["wbr"][L, b].rearrange("(k p) n -> p k n", p=128), writes=[WBR])
    k.dma("gp", WOUT[:], T["w_out"][L].rearrange("(k p) n -> p k n", p=128), writes=[WOUT])
    k.dma("gp", RW[:], T["router_w"].rearrange("(k p) n -> p k n", p=128), writes=[RW])
    gbT = P.sb([128, 24], F32)
    k.dma("sync", gbT[:], T["gate_biasT"][L], writes=[gbT])
    lnp = {}
    for nm in ("ln1_g", "ln1_b", "ln2_g", "ln2_b"):
        lnp[nm] = P.sb([128, D], F32, nm)
        k.dma("sync", lnp[nm][:], T[nm][L].partition_broadcast(128), writes=[lnp[nm]])
    rb = P.sb([128, 16], F32)
    k.dma("sync", rb[:], T["router_bias"].partition_broadcast(128), writes=[rb])
    sc = ln_scratch(P)
    pT = P.ps([128, 1024], BF16, "pT")
    pG_r = Rot([P.ps([128, 512], F32, "pG") for _ in range(3)])
    p1_r = Rot([P.ps([128, 512], F32, "p1") for _ in range(2)])
    p3_r = Rot([P.ps([128, 512], F32, "p3") for _ in range(2)])
    pO_r = pG_r
    yin_r = Rot([P.sb([128, 1536], BF16, "yin") for _ in range(1)])
    yT = P.sb([128, 12, 512], BF16, "yT")
    gl_r = Rot([P.sb([128, 512], BF16, "gl") for _ in range(2)])
    macc = P.sb([128, 512], F32, "macc")
    mtmp_r = Rot([P.sb([128, 512], F32, "mtmp") for _ in range(2)])
    mT = P.sb([128, 8, 512], BF16, "mT")
    xr_r = Rot([P.sb([128, D], F32, "xr") for _ in range(1)])
    xa = P.sb([128, D], F32, "xa")
    X1 = P.sb([128, 4, D], F32, "X1")
    xb_r = Rot([P.sb([128, D], BF16, "xb") for _ in range(2)])
    x1T = P.sb([128, 8, 512], BF16, "x1T")
    ACC = P.sb([128, 4, D], F32, "ACC")
    CW = P.sb([128, 4, 16], F32, "CW")
    w1_r = Rot([P.sb([128, 8, 512], BF16, "w1") for _ in range(2)])
    w3_r = Rot([P.sb([128, 8, 512], BF16, "w3") for _ in range(2)])
    w2_r = Rot([P.sb([128, 4, 1024], BF16, "w2") for _ in range(1)])
    sl_r = Rot([P.sb([128, 512], F32, "sl") for _ in range(2)])
    hm_r = Rot([P.sb([128, 4, 512], BF16, "hm") for _ in range(1)])
    xo_r = Rot([P.sb([128, D], F32, "xo") for _ in range(1)])
    r16 = [P.sb([128, 16], F32, "r16") for _ in range(3)]
    r24 = [P.sb([128, 4, 6], F32, "r24") for _ in range(2)]
    r4 = [P.sb([128, 4], F32, "r4") for _ in range(4)]
    r1 = [P.sb([128, 1], F32, "r1") for _ in range(2)]
    pairs = [(0, 1), (0, 2), (0, 3), (1, 2), (1, 3), (2, 3)]
    dst_out = T["y"] if last else T["xres"]
    for ti in range(DBG.get('ntile', S // 512)):
        ts_ = slice(ti * 512, (ti + 1) * 512)
        for s_ in range(4):
            r0 = ti * 512 + s_ * 128
            yin = yin_r.next()
            k.dma("sync", yin[:], T["Y"][r0:r0 + 128, :], writes=[yin])
            for (c0, n) in ((0, 8), (8, 4)):
                for c in range(n):
                    k.op("pe", lambda e: e.transpose(pT[:, c * 128:(c + 1) * 128], yin[:, (c0 + c) * 128:(c0 + c + 1) * 128], identb[:]), reads=[yin, identb], writes=[pT])
                k.op("dve", lambda e: e.tensor_copy(yT[:, c0:c0 + n, s_ * 128:(s_ + 1) * 128], pT[:, 0:n * 128].rearrange("p (k t) -> p k t", k=n)), reads=[pT], writes=[yT])
        for c in range(8):
            for b in range(3):
                gl = gl_r.next()
                row = R_GATE + b * 1024 + c * 128
                k.dma("sync", gl[:], T["F"][row:row + 128, ts_], writes=[gl])
                k.op("act", lambda e: e.activation(gl[:], gl[:], AF.Sigmoid, bias=gbT[:, b * 8 + c:b * 8 + c + 1]), reads=[gl, gbT], writes=[gl])
                pp = pG_r.next()
                for kk in range(4):
                    k.op("pe", lambda e: e.matmul(pp[:], WBR[:, b * 4 + kk, c * 128:(c + 1) * 128], yT[:, b * 4 + kk, :], start=(kk == 0), stop=(kk == 3)), reads=[WBR, yT], writes=[pp])
                if b == 0:
                    k.op("dve", lambda e: e.tensor_tensor(macc[:], pp[:], gl[:], ALU.mult), reads=[pp, gl], writes=[macc])
                else:
                    mt = mtmp_r.next()
                    k.op("dve", lambda e: e.tensor_tensor(mt[:], pp[:], gl[:], ALU.mult), reads=[pp, gl], writes=[mt])
                    if b == 1:
                        k.op("pool", lambda e: e.tensor_tensor(macc[:], macc[:], mt[:], ALU.add), reads=[macc, mt], writes=[macc])
                    else:
                        k.op("pool", lambda e: e.tensor_tensor(mT[:, c, :], macc[:], mt[:], ALU.add), reads=[macc, mt], writes=[mT])
        for s_ in range(4):
            r0 = ti * 512 + s_ * 128
            xr = xr_r.next()
            k.dma("sync", xr[:], T["xres"][r0:r0 + 128, :], writes=[xr])
            for n in range(2):
                pp = pG_r.next()
                for kk in range(8):
                    k.op("pe", lambda e: e.matmul(pp[:], mT[:, kk, s_ * 128:(s_ + 1) * 128], WOUT[:, kk, n * 512:(n + 1) * 512], start=(kk == 0), stop=(kk == 7)), reads=[mT, WOUT], writes=[pp])
                k.op("dve", lambda e: e.scalar_tensor_tensor(xa[:, n * 512:(n + 1) * 512], xr[:, n * 512:(n + 1) * 512], ALPHA, pp[:], ALU.mult, ALU.add), reads=[xr, pp], writes=[xa])
            layer_norm_tile(k, P, xa, X1s(X1, s_), lnp["ln1_g"], lnp["ln1_b"], sc)
            xb = xb_r.next()
            k.op("act", lambda e: e.activation(xb[:], X1[:, s_, :], AF.Copy), reads=[X1], writes=[xb])
            for kk in range(8):
                k.op("pe", lambda e: e.transpose(pT[:, kk * 128:(kk + 1) * 128], xb[:, kk * 128:(kk + 1) * 128], identb[:]), reads=[xb, identb], writes=[pT])
            k.op("dve", lambda e: e.tensor_copy(x1T[:, :, s_ * 128:(s_ + 1) * 128], pT[:, :].rearrange("p (k t) -> p k t", k=8)), reads=[pT], writes=[x1T])
            pp = pG_r.next()
            for kk in range(8):
                k.op("pe", lambda e: e.matmul(pp[:, 0:16], x1T[:, kk, s_ * 128:(s_ + 1) * 128], RW[:, kk, :], start=(kk == 0), stop=(kk == 7)), reads=[x1T, RW], writes=[pp])
            scr_, bi, ws = r16
            k.op("act", lambda e: e.activation(scr_[:], pp[:, 0:16], AF.Sigmoid), reads=[pp], writes=[scr_])
            k.op("dve", lambda e: e.tensor_tensor(bi[:], scr_[:], rb[:], ALU.add), reads=[scr_, rb], writes=[bi])
            biv = bi[:, :].rearrange("p (g j) -> p g j", g=4)
            PS_, PM_ = r24
            for idx, (a_, b_) in enumerate(pairs):
                k.op("dve", lambda e: e.tensor_tensor(PS_[:, :, idx], biv[:, :, a_], biv[:, :, b_], ALU.add), reads=[bi], writes=[PS_])
                k.op("dve", lambda e: e.tensor_tensor(PM_[:, :, idx], biv[:, :, a_], biv[:, :, b_], ALU.min), reads=[bi], writes=[PM_])
            gs, sec, gsel, _ = r4
            k.op("dve", lambda e: e.reduce_max(gs[:], PS_[:], AX.X), reads=[PS_], writes=[gs])
            k.op("dve", lambda e: e.reduce_max(sec[:], PM_[:], AX.X), reads=[PM_], writes=[sec])
            k.op("dve", lambda e: e.reduce_max(r1[0][:], gs[:], AX.X), reads=[gs], writes=[r1[0]])
            k.op("dve", lambda e: e.tensor_scalar(gsel[:], gs[:], r1[0][:], None, ALU.is_ge), reads=[gs, r1[0]], writes=[gsel])
            wsv = ws[:, :].rearrange("p (g j) -> p g j", g=4)
            for g_ in range(4):
                k.op("dve", lambda e: e.tensor_scalar(wsv[:, g_, :], biv[:, g_, :], sec[:, g_:g_ + 1], gsel[:, g_:g_ + 1], ALU.is_ge, ALU.mult), reads=[bi, sec, gsel], writes=[ws])
            k.op("dve", lambda e: e.tensor_tensor(ws[:], ws[:], scr_[:], ALU.mult), reads=[ws, scr_], writes=[ws])
            k.op("dve", lambda e: e.reduce_sum(r1[1][:], ws[:], AX.X), reads=[ws], writes=[r1[1]])
            k.op("dve", lambda e: e.reciprocal(r1[1][:], r1[1][:]), reads=[r1[1]], writes=[r1[1]])
            k.op("dve", lambda e: e.tensor_scalar(CW[:, s_, :], ws[:], r1[1][:], None, ALU.mult), reads=[ws, r1[1]], writes=[CW])
        for e_ in range(DBG.get('nexp', 16)):
            w1, w3, w2 = w1_r.next(), w3_r.next(), w2_r.next()
            k.dma("sync", w1[:], T["EW1"][e_].rearrange("(k p) n -> p k n", p=128), writes=[w1])
            k.dma("sync", w3[:], T["EW3"][e_].rearrange("(k p) n -> p k n", p=128), writes=[w3])
            k.dma("sync", w2[:], T["EW2"][e_].rearrange("(k p) n -> p k n", p=128), writes=[w2])
            hm = hm_r.next()
            for dc in range(4):
                p1, p3 = p1_r.next(), p3_r.next()
                for kk in range(8):
                    k.op("pe", lambda e: e.matmul(p1[:], w1[:, kk, dc * 128:(dc + 1) * 128], x1T[:, kk, :], start=(kk == 0), stop=(kk == 7)), reads=[w1, x1T], writes=[p1])
                for kk in range(8):
                    k.op("pe", lambda e: e.matmul(p3[:], w3[:, kk, dc * 128:(dc + 1) * 128], x1T[:, kk, :], start=(kk == 0), stop=(kk == 7)), reads=[w3, x1T], writes=[p3])
                sl = sl_r.next()
                k.op("act", lambda e: e.activation(sl[:], p1[:], AF.Silu), reads=[p1], writes=[sl])
                k.op("dve", lambda e: e.tensor_tensor(hm[:, dc, :], sl[:], p3[:], ALU.mult), reads=[sl, p3], writes=[hm])
            for s_ in range(4):
                for n in range(2):
                    po = pO_r.next()
                    for dc in range(4):
                        k.op("pe", lambda e: e.matmul(po[:], hm[:, dc, s_ * 128:(s_ + 1) * 128], w2[:, dc, n * 512:(n + 1) * 512], start=(dc == 0), stop=(dc == 3)), reads=[hm, w2], writes=[po])
                    asl = ACC[:, s_, n * 512:(n + 1) * 512]
                    if e_ == 0:
                        k.op("dve", lambda e: e.tensor_scalar(asl, po[:], CW[:, s_, 0:1], None, ALU.mult), reads=[po, CW], writes=[ACC])
                    else:
                        k.op("dve", lambda e: e.scalar_tensor_tensor(asl, po[:], CW[:, s_, e_:e_ + 1], asl, ALU.mult, ALU.add), reads=[po, CW, ACC], writes=[ACC])
        for s_ in range(4):
            r0 = ti * 512 + s_ * 128
            k.op("dve", lambda e: e.scalar_tensor_tensor(xa[:], X1[:, s_, :], ALPHA, ACC[:, s_, :], ALU.mult, ALU.add), reads=[X1, ACC], writes=[xa])
            xo = xo_r.next()
            layer_norm_tile(k, P, xa, xo, lnp["ln2_g"], lnp["ln2_b"], sc)
            k.dma("sync", dst_out[r0:r0 + 128, :], xo[:], reads=[xo])
    k.new_epoch()
    P.close()


class X1s:
    def __init__(self, buf, s_):
        self._b = buf
        self._s = s_

    def __getitem__(self, idx):
        return self._b.ap[:, self._s, :]

    @property
    def w(self):
        return self._b.w

    @w.setter
    def w(self, v):
        self._b.w = v

    @property
    def r(self):
        return self._b.r

    @r.setter
    def r(self, v):
        self._b.r = v


def build(dbg=None, phases=("p1",), nlayers=DEPTH):
    nc = bass.Bass("TRN2", target_bir_lowering=False)
    T = {}

    def inp(name, shape, dt=F32):
        T[name] = nc.dram_tensor(name, list(shape), dt, kind="ExternalInput").ap()

    def scr(name, shape, dt):
        kind = "ExternalOutput" if (dbg and name in dbg) else "Internal"
        if name in DBG.get("ext_in", ()):
            kind = "ExternalInput"
        T[name] = nc.dram_tensor(name, list(shape), dt, kind=kind).ap()

    inp("x", [S, D])
    inp("win", [DEPTH, D, NW])
    inp("ln_in_g", [D])
    inp("ln_in_b", [D])
    inp("c_ident", [128, 128])
    inp("c_mats", [128, 7, 128])
    inp("gdn_a_log", [DEPTH, 4])
    inp("gdn_dt_bias", [DEPTH, 4])
    inp("gdn_norm", [DEPTH, 128])
    inp("gdn_convT", [DEPTH, 128, 12, 4])
    inp("mlstm_gate_bias", [DEPTH, 8])
    inp("mlstm_norm", [DEPTH, 512])
    inp("wuq", [DEPTH, 384, 1024])
    inp("mla_w_ukv", [DEPTH, 256, 1024])
    inp("qnormT", [DEPTH, 128, 3])
    inp("kvnormT", [DEPTH, 128, 2])
    inp("c_rope", [64, 3])
    inp("c_amask", [128, 4, 512])
    inp("positions", [S], I32)
    inp("wbr", [DEPTH, 3, 512, 1024])
    inp("w_out", [DEPTH, D, D])
    inp("router_w", [D, 16])
    inp("router_bias", [16])
    inp("gate_biasT", [DEPTH, 128, 24])
    for nm in ("ln1_g", "ln1_b", "ln2_g", "ln2_b"):
        inp(nm, [DEPTH, D])
    inp("moe_w1", [DEPTH, 16, D, 512])
    inp("moe_w3", [DEPTH, 16, D, 512])
    inp("moe_w2", [DEPTH, 16, 512, D])
    T["y"] = nc.dram_tensor("y", [S, D], F32, kind="ExternalOutput").ap()
    scr("xres", [S, D], F32)
    scr("F", [NFEAT, S], BF16)
    scr("Th", [S, NTOK], BF16)
    scr("Ts", [S, 16], F32)
    scr("Y", [S, 1536], BF16)
    scr("MQ", [4, 192, S], BF16)
    scr("MK", [4, 128, S], BF16)
    scr("MKR", [64, S], BF16)
    scr("MV", [4, S, 128], BF16)
    scr("EW1", [16, D, 512], BF16)
    scr("EW3", [16, D, 512], BF16)
    scr("EW2", [16, 512, D], BF16)
    k = K(nc)
    for L in range(nlayers):
        if "p1" in phases:
            phase1(k, nc, T, L)
        if "p2" in phases:
            phase2(k, nc, T, L)
        if "p3" in phases:
            phase3(k, nc, T, L)
        if "p4" in phases:
            phase4(k, nc, T, L)
        if "p5" in phases:
            phase0(k, nc, T, L)
            phase5(k, nc, T, L, last=(L == nlayers - 1))
    k.finish()
    return nc


def const_mats():
    a = np.arange(128)[:, None]
    b = np.arange(128)[None, :]
    m = np.zeros((128, 7, 128), np.float32)
    m[:, 0] = (a == b)
    m[:, 1] = (a <= b)
    m[:, 2] = (a > b)
    m[:, 3] = (a < b)
    m[:, 4] = 1.0
    m[:, 5] = np.where(a < b, -1e30, 0.0)
    m[:, 6] = (a == 127)
    return m


def perm_wuq(w):
    idx = []
    for h in range(4):
        b = h * 192
        idx += list(range(b, b + 128)) + list(range(b + 128, b + 192)) + list(range(b + 160, b + 192)) + list(range(b + 128, b + 160))
    return np.ascontiguousarray(w[:, :, np.array(idx)])


def rope_consts():
    f = np.arange(32, dtype=np.float32)
    inv = (1.0 / (np.float32(10000.0) ** (f * 2 / 64))).astype(np.float32)
    c = np.zeros((64, 3), np.float32)
    c[:, 0] = np.concatenate([inv, inv])
    c[:, 1] = math.pi / 2
    c[:32, 2] = math.pi
    c[32:, 2] = 0.0
    return c


def attn_masks():
    kk = np.arange(128)[:, None, None]
    jj = np.arange(4)[None, :, None]
    qq = np.arange(512)[None, None, :]
    return (qq >= kk + 128 * jj).astype(np.float32)


def perm_win(w_in):
    o = {}
    names = ["g_q", "g_k", "g_v", "g_z", "g_a", "g_b", "m_q", "m_k", "m_v", "m_o", "m_i", "m_f", "c_q", "c_kv", "k_rope", "gate"]
    sizes = [512, 512, 512, 512, 4, 4, 512, 512, 512, 512, 4, 4, 384, 256, 64, 3072]
    a = 0
    for n, s in zip(names, sizes):
        o[n] = np.arange(a, a + s)
        a += s
    kr = o["k_rope"]
    krs = np.concatenate([kr[32:], kr[:32]])
    idx = np.concatenate([o["g_q"], o["g_k"], o["g_v"], o["m_q"], o["m_k"], o["c_q"], o["c_kv"], kr, krs, o["gate"],
                          o["g_z"], o["m_v"], o["m_o"], o["g_a"], o["g_b"], o["m_i"], o["m_f"]])
    assert idx.size == NW
    return np.ascontiguousarray(w_in[:, :, idx])


def host_inputs(inputs):
    common = {
        "win": perm_win(np.asarray(inputs["w_in"], np.float32)),
        "ln_in_g": np.asarray(inputs["ln_in_g"], np.float32),
        "ln_in_b": np.asarray(inputs["ln_in_b"], np.float32),
        "c_ident": np.eye(128, dtype=np.float32),
        "c_mats": const_mats(),
        "gdn_a_log": np.asarray(inputs["gdn_a_log"], np.float32),
        "gdn_dt_bias": np.asarray(inputs["gdn_dt_bias"], np.float32),
        "gdn_norm": np.asarray(inputs["gdn_norm"], np.float32),
        "mlstm_gate_bias": np.asarray(inputs["mlstm_gate_bias"], np.float32),
        "mlstm_norm": np.asarray(inputs["mlstm_norm"], np.float32),
        "wuq": perm_wuq(np.asarray(inputs["mla_w_uq"], np.float32)),
        "mla_w_ukv": np.asarray(inputs["mla_w_ukv"], np.float32),
        "qnormT": np.ascontiguousarray(np.asarray(inputs["mla_q_norm"], np.float32).reshape(DEPTH, 3, 128).transpose(0, 2, 1)),
        "kvnormT": np.ascontiguousarray(np.asarray(inputs["mla_kv_norm"], np.float32).reshape(DEPTH, 2, 128).transpose(0, 2, 1)),
        "c_rope": rope_consts(),
        "c_amask": attn_masks(),
        "wbr": np.ascontiguousarray(np.stack([np.asarray(inputs[n], np.float32) for n in ("w_br_gdn", "w_br_mlstm", "w_br_mla")], axis=1)),
        "w_out": np.asarray(inputs["w_out"], np.float32),
        "router_w": np.asarray(inputs["router_w"], np.float32),
        "router_bias": np.asarray(inputs["router_bias"], np.float32),
        "gate_biasT": np.ascontiguousarray(np.asarray(inputs["gate_bias"], np.float32).reshape(DEPTH, 24, 128).transpose(0, 2, 1)),
        "ln1_g": np.asarray(inputs["ln1_g"], np.float32), "ln1_b": np.asarray(inputs["ln1_b"], np.float32),
        "ln2_g": np.asarray(inputs["ln2_g"], np.float32), "ln2_b": np.asarray(inputs["ln2_b"], np.float32),
        "moe_w1": np.asarray(inputs["moe_w1"], np.float32), "moe_w3": np.asarray(inputs["moe_w3"], np.float32),
        "moe_w2": np.asarray(inputs["moe_w2"], np.float32),
        "gdn_convT": np.ascontiguousarray(np.asarray(inputs["gdn_conv"], np.float32).reshape(DEPTH, 4, 12, 128).transpose(0, 3, 2, 1)),
    }
    maps = []
    for c in range(8):
        m = dict(common)
        m["x"] = np.ascontiguousarray(inputs["x"][c % 4])
        m["positions"] = np.ascontiguousarray(inputs["positions"][c % 4]).astype(np.int32)
        maps.append(m)
    return maps


def kernel(**inputs):
    nc = build(phases=("p1", "p2", "p3", "p4", "p5"))
    maps = host_inputs(inputs)
    res = run_bass_kernel_spmd(nc, maps, core_ids=list(range(8)))
    out = np.stack([res.results[c]["y"] for c in range(4)], axis=0)
    return out.astype(np.float32)
```
